# Optimizing a Trainium2 kernel written in Bass

```python
import math
import jax, jax.numpy as jnp
from jax import lax
import numpy as np

D_MODEL = 2048
BATCH = 4
SEQ = 4096
DEPTH = 1

CHUNK = 64
MIX_WIDTH = D_MODEL
S5_WIDTH = MIX_WIDTH // 2
S5_GROUP_WIDTH = 16
S5_GROUPS = S5_WIDTH // S5_GROUP_WIDTH
S5_STATE = 64
S5_DT_MIN = 1e-3
S5_DT_MAX = 1e-1
RWKV_WIDTH = MIX_WIDTH - S5_WIDTH
RWKV_HEAD = 64
RWKV_HEADS = RWKV_WIDTH // RWKV_HEAD
RWKV_DECAY_LORA = max(32, int(round(1.8 * RWKV_WIDTH ** 0.5 / 32)) * 32)
RWKV_AAA_LORA = max(32, int(round(1.8 * RWKV_WIDTH ** 0.5 / 32)) * 32)
RWKV_GATE_LORA = max(32, int(round(0.6 * RWKV_WIDTH ** 0.8 / 32)) * 32)
RWKV_SPLITS = (RWKV_WIDTH, RWKV_WIDTH, RWKV_WIDTH, RWKV_DECAY_LORA, RWKV_AAA_LORA, RWKV_GATE_LORA)
RWKV_PROJ = sum(RWKV_SPLITS)
PROJ_WIDTH = S5_WIDTH + RWKV_PROJ
RWKV_GN_EPS = 64e-5
N_GROUPS = 8
EXPERTS_PER_GROUP = 8
N_EXPERTS = N_GROUPS * EXPERTS_PER_GROUP
TOP_K = 2
D_EXPERT = D_MODEL // 4
MOE_BLOCK = 256
RMS_EPS = 1e-6

kernel_name = "hymba_s5_rwkv7_hiermoe_block"

F32 = jnp.float32


def rms_norm(x, g):
    xf = x.astype(F32)
    y = xf * lax.rsqrt(jnp.mean(xf * xf, axis=-1, keepdims=True) + RMS_EPS)
    return (y * g.astype(F32)).astype(x.dtype)


def cmul(ar, ai, br, bi):
    return ar * br - ai * bi, ar * bi + ai * br


def linear_recurrence_combine(e_i, e_j):
    ai_r, ai_i, bi_r, bi_i = e_i
    aj_r, aj_i, bj_r, bj_i = e_j
    a_r, a_i = cmul(aj_r, aj_i, ai_r, ai_i)
    t_r, t_i = cmul(aj_r, aj_i, bi_r, bi_i)
    return a_r, a_i, t_r + bj_r, t_i + bj_i


def s5_mixer(u, lam_re, lam_im, log_step, b_re, b_im, c_re, c_im, d_skip, w_glu, b_glu):
    bsz, seq, width = u.shape
    n_chunks = seq // CHUNK
    G, P = S5_GROUPS, S5_STATE
    uf = u.astype(F32).reshape(bsz, n_chunks, CHUNK, G, S5_GROUP_WIDTH)
    lr = lam_re.astype(F32)
    li = lam_im.astype(F32)
    dt = jnp.exp(log_step.astype(F32))[:, None]
    mag = jnp.exp(lr * dt)
    ang = li * dt
    abar_re, abar_im = mag * jnp.cos(ang), mag * jnp.sin(ang)
    den = lr * lr + li * li
    num_re, num_im = abar_re - 1.0, abar_im
    f_re = (num_re * lr + num_im * li) / den
    f_im = (num_im * lr - num_re * li) / den
    bb_re, bb_im = cmul(f_re[..., None], f_im[..., None], b_re.astype(F32), b_im.astype(F32))
    bu_re = jnp.einsum('bctgh,gph->bctgp', uf, bb_re)
    bu_im = jnp.einsum('bctgh,gph->bctgp', uf, bb_im)
    a_shape = (1, 1, CHUNK, G, P)
    _, _, loc_re, loc_im = lax.associative_scan(
        linear_recurrence_combine,
        (jnp.broadcast_to(abar_re, a_shape), jnp.broadcast_to(abar_im, a_shape), bu_re, bu_im),
        axis=2)
    tau = jnp.arange(1, CHUNK + 1, dtype=F32)[:, None, None]
    pmag = jnp.exp(lr * dt * tau)
    pang = li * dt * tau
    apow_re, apow_im = pmag * jnp.cos(pang), pmag * jnp.sin(pang)
    c_shape = (1, n_chunks, G, P)
    _, _, car_re, car_im = lax.associative_scan(
        linear_recurrence_combine,
        (jnp.broadcast_to(apow_re[-1], c_shape), jnp.broadcast_to(apow_im[-1], c_shape),
         loc_re[:, :, -1], loc_im[:, :, -1]),
        axis=1)
    pad = ((0, 0), (1, 0), (0, 0), (0, 0))
    prev_re = jnp.pad(car_re, pad)[:, :-1]
    prev_im = jnp.pad(car_im, pad)[:, :-1]
    cor_re, cor_im = cmul(apow_re, apow_im, prev_re[:, :, None], prev_im[:, :, None])
    s_re = loc_re + cor_re
    s_im = loc_im + cor_im
    y = (jnp.einsum('ghp,bctgp->bctgh', c_re.astype(F32), s_re)
         - jnp.einsum('ghp,bctgp->bctgh', c_im.astype(F32), s_im))
    y = y.reshape(bsz, seq, width) + d_skip.astype(F32) * u.astype(F32)
    y = jax.nn.gelu(y)
    y = y * jax.nn.sigmoid(y @ w_glu.astype(F32) + b_glu.astype(F32))
    return y.astype(u.dtype)


def token_shift_lerp(z, mu):
    prev = jnp.pad(z, ((0, 0), (1, 0), (0, 0)))[:, :-1]
    return z + (prev - z) * mu


def rwkv7_recurrence(r, w, k, v, a_vec, b_vec):
    bsz, _, H, N = r.shape

    def step(S, inp):
        r_t, w_t, k_t, v_t, a_t, b_t = inp
        sa = jnp.einsum('bhvk,bhk->bhv', S, a_t)
        S = S * w_t[:, :, None, :] + sa[..., None] * b_t[:, :, None, :] + v_t[..., None] * k_t[:, :, None, :]
        return S, jnp.einsum('bhvk,bhk->bhv', S, r_t)

    xs = tuple(jnp.moveaxis(t, 1, 0) for t in (r, w, k, v, a_vec, b_vec))
    _, y = lax.scan(step, jnp.zeros((bsz, H, N, N), F32), xs)
    return jnp.moveaxis(y, 0, 1)


def rwkv7_mixer(z, mu, w0, w2, a0, a2, g2, k_k, k_a, r_k, ln_w, ln_b):
    bsz, seq, _ = z.shape
    H, N = RWKV_HEADS, RWKV_HEAD
    zs = token_shift_lerp(z, mu).astype(F32)
    r, k, v, xw, xa, xg = jnp.split(zs, np.cumsum(RWKV_SPLITS)[:-1].tolist(), axis=-1)
    w_log = -jax.nn.softplus(-(w0.astype(F32) + jnp.tanh(xw) @ w2.astype(F32))) - 0.5
    decay = jnp.exp(-jnp.exp(w_log))
    a = jax.nn.sigmoid(a0.astype(F32) + xa @ a2.astype(F32))
    g = jax.nn.sigmoid(xg) @ g2.astype(F32)
    heads = lambda t: t.reshape(bsz, seq, H, N)
    kk = heads(k * k_k.astype(F32))
    kk = kk / jnp.maximum(jnp.sqrt(jnp.sum(kk * kk, axis=-1, keepdims=True)), 1e-12)
    k = k * (1.0 + (a - 1.0) * k_a.astype(F32))
    rh, kh, vh = heads(r), heads(k), heads(v)
    y = rwkv7_recurrence(rh, heads(decay), kh, vh, -kk, kk * heads(a))
    mean = jnp.mean(y, axis=-1, keepdims=True)
    var = jnp.mean(jnp.square(y - mean), axis=-1, keepdims=True)
    yn = ((y - mean) * lax.rsqrt(var + RWKV_GN_EPS)).reshape(bsz, seq, RWKV_WIDTH)
    yn = yn * ln_w.astype(F32) + ln_b.astype(F32)
    bonus = jnp.sum(rh * kh * r_k.astype(F32), axis=-1, keepdims=True) * vh
    out = (yn + bonus.reshape(bsz, seq, RWKV_WIDTH)) * g
    return out.astype(z.dtype)


def hier_moe(h, w_route_grp, b_route_grp, w_route_exp, b_route_exp, w_gate, w_up, w_down):
    bsz, seq, D = h.shape
    T = bsz * seq
    xt = h.reshape(T, D)
    grp_prob = jax.nn.softmax((xt @ w_route_grp).astype(F32) + b_route_grp.astype(F32), axis=-1)
    grp_p, grp_idx = lax.top_k(grp_prob, 1)
    exp_logits = ((xt @ w_route_exp).astype(F32) + b_route_exp.astype(F32)).reshape(T, N_GROUPS, EXPERTS_PER_GROUP)
    sel_logits = jnp.take_along_axis(exp_logits, grp_idx[:, :, None], axis=1)[:, 0]
    top_p, top_j = lax.top_k(jax.nn.softmax(sel_logits, axis=-1), TOP_K)
    top_p = top_p / jnp.sum(top_p, axis=-1, keepdims=True)
    gate = (grp_p * top_p).reshape(-1)
    eid = (grp_idx * EXPERTS_PER_GROUP + top_j).reshape(-1).astype(jnp.int32)
    tok = jnp.repeat(jnp.arange(T, dtype=jnp.int32), TOP_K)
    S = T * TOP_K
    order = jnp.argsort(eid)
    e_sorted = eid[order]
    counts = jnp.bincount(eid, length=N_EXPERTS)
    padded = (counts + MOE_BLOCK - 1) // MOE_BLOCK * MOE_BLOCK
    start = jnp.cumsum(counts) - counts
    pend = jnp.cumsum(padded)
    pstart = pend - padded
    dest = pstart[e_sorted] + jnp.arange(S, dtype=jnp.int32) - start[e_sorted]
    n_blocks = -(-S // MOE_BLOCK) + N_EXPERTS
    rows = n_blocks * MOE_BLOCK
    buf_tok = jnp.full((rows,), T, jnp.int32).at[dest].set(tok[order])
    buf_gate = jnp.zeros((rows,), F32).at[dest].set(gate[order])
    blk_exp = jnp.minimum(jnp.searchsorted(pend, jnp.arange(n_blocks) * MOE_BLOCK, side='right'),
                          N_EXPERTS - 1).astype(jnp.int32)
    x_pad = jnp.concatenate([xt, jnp.zeros((1, D), xt.dtype)], axis=0)
    xb = x_pad[buf_tok].reshape(n_blocks, MOE_BLOCK, D)

    def expert_block(args):
        xblk, e = args
        return (jax.nn.silu(xblk @ w_gate[e]) * (xblk @ w_up[e])) @ w_down[e]

    yb = lax.map(expert_block, (xb, blk_exp)).reshape(rows, D).astype(F32) * buf_gate[:, None]
    y = jnp.zeros((T + 1, D), F32).at[buf_tok].add(yb)[:T]
    return y.reshape(bsz, seq, D).astype(h.dtype)


def setup_inputs(seed: int = 0) -> dict:
    key = jax.random.key(seed)
    keys = jax.random.split(key, 48)
    cnt = [0]

    def nk():
        cnt[0] += 1
        return keys[cnt[0] - 1]

    def nrm(shape, scale):
        return jax.random.normal(nk(), shape, F32) * scale

    def uni(shape, lo, hi):
        return jax.random.uniform(nk(), shape, F32, lo, hi)

    L, G, P, Hg = DEPTH, S5_GROUPS, S5_STATE, S5_GROUP_WIDTH
    RW, NH, N = RWKV_WIDTH, RWKV_HEADS, RWKV_HEAD
    return {
        "x": nrm((BATCH, SEQ, D_MODEL), 1.0),
        "norm_mix_g": 1.0 + nrm((L, D_MODEL), 0.02),
        "w_in": nrm((L, D_MODEL, PROJ_WIDTH), D_MODEL ** -0.5),
        "s5_lambda_re": -0.5 + nrm((L, G, P), 0.01),
        "s5_lambda_im": jnp.pi * jnp.arange(P, dtype=F32) + nrm((L, G, P), 0.01),
        "s5_log_step": uni((L, G), math.log(S5_DT_MIN), math.log(S5_DT_MAX)),
        "s5_b_re": nrm((L, G, P, Hg), (2 * Hg) ** -0.5),
        "s5_b_im": nrm((L, G, P, Hg), (2 * Hg) ** -0.5),
        "s5_c_re": nrm((L, G, Hg, P), P ** -0.5),
        "s5_c_im": nrm((L, G, Hg, P), P ** -0.5),
        "s5_d": nrm((L, S5_WIDTH), 1.0),
        "s5_w_glu": nrm((L, S5_WIDTH, S5_WIDTH), S5_WIDTH ** -0.5),
        "s5_b_glu": nrm((L, S5_WIDTH), 0.01),
        "rwkv_mu": uni((L, RWKV_PROJ), 0.0, 1.0),
        "rwkv_w0": uni((L, RW), -5.0, -1.0),
        "rwkv_w2": nrm((L, RWKV_DECAY_LORA, RW), 0.1),
        "rwkv_a0": nrm((L, RW), 0.1),
        "rwkv_a2": nrm((L, RWKV_AAA_LORA, RW), 0.1),
        "rwkv_g2": nrm((L, RWKV_GATE_LORA, RW), RWKV_GATE_LORA ** -0.5),
        "rwkv_k_k": 0.85 + nrm((L, RW), 0.02),
        "rwkv_k_a": 1.0 + nrm((L, RW), 0.02),
        "rwkv_r_k": nrm((L, NH, N), 0.1),
        "rwkv_ln_w": 1.0 + nrm((L, RW), 0.02),
        "rwkv_ln_b": nrm((L, RW), 0.01),
        "w_out": nrm((L, MIX_WIDTH, D_MODEL), MIX_WIDTH ** -0.5),
        "norm_ffn_g": 1.0 + nrm((L, D_MODEL), 0.02),
        "w_route_grp": nrm((L, D_MODEL, N_GROUPS), D_MODEL ** -0.5),
        "b_route_grp": nrm((L, N_GROUPS), 0.01),
        "w_route_exp": nrm((L, D_MODEL, N_EXPERTS), D_MODEL ** -0.5),
        "b_route_exp": nrm((L, N_EXPERTS), 0.01),
        "w_gate": nrm((L, N_EXPERTS, D_MODEL, D_EXPERT), D_MODEL ** -0.5),
        "w_up": nrm((L, N_EXPERTS, D_MODEL, D_EXPERT), D_MODEL ** -0.5),
        "w_down": nrm((L, N_EXPERTS, D_EXPERT, D_MODEL), D_EXPERT ** -0.5),
        "norm_final_g": 1.0 + nrm((D_MODEL,), 0.02),
    }


def reference(x, norm_mix_g, w_in, s5_lambda_re, s5_lambda_im, s5_log_step, s5_b_re, s5_b_im,
              s5_c_re, s5_c_im, s5_d, s5_w_glu, s5_b_glu, rwkv_mu, rwkv_w0, rwkv_w2, rwkv_a0,
              rwkv_a2, rwkv_g2, rwkv_k_k, rwkv_k_a, rwkv_r_k, rwkv_ln_w, rwkv_ln_b, w_out,
              norm_ffn_g, w_route_grp, b_route_grp, w_route_exp, b_route_exp, w_gate, w_up,
              w_down, norm_final_g):
    h = x
    for l in range(DEPTH):
        hn = rms_norm(h, norm_mix_g[l])
        proj = hn @ w_in[l]
        u_s5 = proj[..., :S5_WIDTH]
        z_rw = proj[..., S5_WIDTH:]
        y_s5 = s5_mixer(u_s5, s5_lambda_re[l], s5_lambda_im[l], s5_log_step[l], s5_b_re[l],
                        s5_b_im[l], s5_c_re[l], s5_c_im[l], s5_d[l], s5_w_glu[l], s5_b_glu[l])
        y_rw = rwkv7_mixer(z_rw, rwkv_mu[l], rwkv_w0[l], rwkv_w2[l], rwkv_a0[l], rwkv_a2[l],
                           rwkv_g2[l], rwkv_k_k[l], rwkv_k_a[l], rwkv_r_k[l], rwkv_ln_w[l],
                           rwkv_ln_b[l])
        mixed = jnp.concatenate([y_s5.astype(h.dtype), y_rw.astype(h.dtype)], axis=-1)
        h = h + mixed @ w_out[l]
        h = h + hier_moe(rms_norm(h, norm_ffn_g[l]), w_route_grp[l], b_route_grp[l],
                         w_route_exp[l], b_route_exp[l], w_gate[l], w_up[l], w_down[l])
    return rms_norm(h, norm_final_g)
```

```python
import contextlib
import numpy as np
import ml_dtypes
import concourse.bass as bass
import concourse.mybir as mybir
from concourse.bass_utils import run_bass_kernel_spmd

F32 = mybir.dt.float32
BF16 = mybir.dt.bfloat16
I32 = mybir.dt.int32
U32 = mybir.dt.uint32
AF = mybir.ActivationFunctionType
ALU = mybir.AluOpType
AX = mybir.AxisListType

ENGS = ("pe", "act", "dve", "pool", "sp")
EPOCH = 2000
EPOCH_DMA = 250


class Ctr:
    def __init__(self, s, name, inc):
        self.s, self.name, self.inc, self.n, self.sems = s, name, inc, 0, []
        self.E = EPOCH if inc == 1 else EPOCH_DMA

    def tick(self):
        EPOCH = self.E
        ep = self.n // EPOCH
        if ep >= len(self.sems):
            self.sems.append(self.s.es.enter_context(self.s.nc.semaphore(f"{self.name}_{ep}")))
        self.n += 1
        v = (self.n - ep * EPOCH) * self.inc
        return (self.name, ep), self.sems[ep], v

    def cur(self):
        if self.n == 0:
            return None
        EPOCH = self.E
        ep = (self.n - 1) // EPOCH
        return (self.name, ep), self.sems[ep], (self.n - ep * EPOCH) * self.inc


class _Rec:
    def __init__(self):
        self.call = None

    def __getattr__(self, name):
        def f(*a, **k):
            self.call = (name, a, k)
        return f


def _record(fn):
    r = _Rec()
    fn(r)
    assert r.call is not None
    return r.call


class Sched:
    def __init__(self, nc, es):
        self.nc, self.es = nc, es
        self.e = dict(pe=nc.tensor, act=nc.scalar, dve=nc.vector, pool=nc.gpsimd, sp=nc.sync)
        self.ctr = {k: Ctr(self, "e" + k, 1) for k in ENGS}
        self.dctr = {}
        self.prog = {k: [] for k in ENGS}
        self.seen = {k: {} for k in ENGS}
        self.tiles = {}
        self.semh = {}
        self.nins = 0
        self.log = None

    def _wait(self, eng, dep):
        semkey, sem, val = dep
        if semkey[0][0] == "d":
            c = self.dctr[semkey[0][1:]]
            cur = c.cur()
            val = cur[2] if cur[0] == semkey else EPOCH_DMA * 16
        if self.seen[eng].get(semkey, 0) >= val:
            return
        self.seen[eng][semkey] = val
        self.prog[eng].append(("w", sem, val))
        if self.log is not None:
            self.log.append(f"   {eng} WAIT {semkey} >= {val}")

    def _deps(self, eng, R, W):
        deps = []
        for k in R:
            t = self.tiles.get(k)
            if t and t[0]:
                deps.append(t[0])
        for k in W:
            t = self.tiles.get(k)
            if t:
                if t[0]:
                    deps.append(t[0])
                deps.extend(t[1].values())
        for d in deps:
            if eng == "pe" and d[0][0] == "epe":
                continue
            self._wait(eng, d)

    def _commit(self, me, R, W):
        for k in R:
            t = self.tiles.setdefault(k, [None, {}])
            t[1][me[0]] = me
        for k in W:
            self.tiles[k] = [me, {}]

    def op(self, eng, fn, R=(), W=(), sync_prev=False):
        self._deps(eng, R, W)
        if sync_prev and self.ctr[eng].cur() is not None:
            self._wait(eng, self.ctr[eng].cur())
        me = self.ctr[eng].tick()
        if self.log is not None:
            self.log.append(f"{eng} #{me[2]} ep{me[0][1]} R={list(R)} W={list(W)}")
        self.prog[eng].append(("i", _record(fn), me[1], 1))
        self._commit(me, R, W)
        self.nins += 1

    def dma(self, q, out, in_, R=(), W=(), stream="d0", **kw):
        stream = stream + "_" + q
        self._deps(q, R, W)
        c = self.dctr.get(stream)
        if c is None:
            c = self.dctr[stream] = Ctr(self, "d" + stream, 16)
        me = c.tick()
        self.prog[q].append(("i", ("dma_start", (), dict(out=out, in_=in_, **kw)), me[1], 16))
        self._commit(me, R, W)
        self.nins += 1

    def dmafn(self, q, fn, R=(), W=(), stream="d0"):
        stream = stream + "_" + q
        self._deps(q, R, W)
        c = self.dctr.get(stream)
        if c is None:
            c = self.dctr[stream] = Ctr(self, "d" + stream, 16)
        me = c.tick()
        self.prog[q].append(("i", _record(fn), me[1], 16))
        self._commit(me, R, W)
        self.nins += 1

    def barrier(self):
        cs = [c.cur() for c in list(self.ctr.values()) + list(self.dctr.values())]
        for eng in ENGS:
            for d in cs:
                if d is not None:
                    self._wait(eng, d)
        self.tiles = {}

    def flush(self):
        prog = self.prog
        self.prog = {k: [] for k in ENGS}
        with self.nc.Block() as block:
            def run(items):
                def f(e):
                    for it in items:
                        if it[0] == "w":
                            e.wait_ge(it[1], it[2])
                        else:
                            name, a, kw = it[1]
                            getattr(e, name)(*a, **kw).then_inc(it[2], it[3])
                return f
            block.tensor(run(prog["pe"]))
            block.scalar(run(prog["act"]))
            block.vector(run(prog["dve"]))
            block.gpsimd(run(prog["pool"]))
            block.sync(run(prog["sp"]))


D = 2048
TP = 1024
NPASS = 4
TF = 2048
NT = 512
PROJ = 4384
CAP = 384
TWO_PI = 6.283185307179586

DEBUG = {}

IN_SPECS = [
    ("xT", (D, 4096)), ("xtok", (TF, D)), ("gmix", (128, 16)), ("w_in", (D, PROJ)),
    ("mu_bc", (128, 3360)), ("lam_re", (128, 32)), ("lam_im", (128, 32)), ("lstep", (128, 32)),
    ("Bx_re", (128, 32 * 128)), ("Bx_im", (128, 32 * 128)), ("Cx_re", (128, 32 * 128)), ("Cx_im", (128, 32 * 128)),
    ("s5d", (128, 8)), ("w_glu", (1024, 1024)), ("bglu", (128, 8)), ("tidx", (128, NT)),
    ("w0", (128, 8)), ("a0", (128, 8)), ("w2", (64, 1024)), ("a2", (64, 1024)), ("g2", (160, 1024)),
    ("k_k", (128, 8)), ("k_a", (128, 8)), ("r_k", (128, 8)), ("lnw_bc", (64, 1024)), ("lnb_bc", (64, 1024)),
    ("w_out", (D, D)), ("gffn_bc", (128, D)), ("wr", (D, 72)), ("br_bc", (128, 72)),
    ("w_gate", (64, D, 512)), ("w_up", (64, D, 512)), ("w_down", (64, 512, D)), ("gfin_bc", (128, D)),
    ("ident", (128, 128)), ("maskLT", (64, 512)), ("maskLE", (64, 512)), ("maskGT", (64, 512)),
    ("identrep", (64, 512)), ("rstmask", (128, NT)), ("bones", (128, 128)), ("hsel", (128, 2)),
    ("ltri", (128, 128)), ("gbase", (128, 8)), ("gffn_pc", (128, 16)),
]


def build_program(dbg=None, stages=("s5", "rwkv", "moe")):
    nc = bass.Bass("TRN2", target_bir_lowering=False)
    es = contextlib.ExitStack()
    s = Sched(nc, es)
    op, dma = s.op, s.dma
    I = {name: nc.dram_tensor(name, list(shape), F32, kind="ExternalInput").ap() for name, shape in IN_SPECS
         if "moe" in stages or name not in ("w_gate", "w_up", "w_down")}
    out = nc.dram_tensor("out", [TF, D], F32, kind="ExternalOutput").ap()
    DBG = {}
    for name, shape in (dbg or {}).items():
        DBG[name] = nc.dram_tensor(name, list(shape), F32, kind="ExternalOutput").ap()
    h_scr = nc.dram_tensor("h_scr", [TF, D], F32, kind="Internal").ap()
    hn_scr = nc.dram_tensor("hn_scr", [8 * CAP, D], BF16, kind="Internal").ap()
    gt_scr = nc.dram_tensor("gt_scr", [8 * CAP, 8], F32, kind="Internal").ap()
    y_scr = nc.dram_tensor("y_scr", [8 * CAP, D], F32, kind="Internal").ap()

    uniq = [0]

    def sb(st, name, shape, dt=F32):
        uniq[0] += 1
        return st.enter_context(nc.sbuf_tensor(f"{name}_u{uniq[0]}", list(shape), dt))

    PS = [es.enter_context(nc.psum_tensor(f"ps{i}", [128, 512], F32)) for i in range(8)]
    psi = [0]
    pinned = set()

    def ps(pin=False):
        while psi[0] in pinned:
            psi[0] = (psi[0] + 1) % 8
        i = psi[0]
        psi[0] = (i + 1) % 8
        if pin:
            pinned.add(i)
        return PS[i], f"ps{i}"

    def unpin(pk):
        pinned.discard(int(pk[2:]))

    def phase_end():
        s.barrier()
        s.flush()

    def tap(name, ap, R):
        if name in DBG:
            dma("pool", DBG[name], ap, R=R, stream="dbg")

    dest_i = sb(es, "dest_i", [128, 16], I32)
    ident = sb(es, "identb", [128, 128], BF16)
    identf = sb(es, "identf", [128, 128], F32)
    dma("pool", ident[:], I["ident"], W=["identb"], stream="c")
    dma("sp", identf[:], I["ident"], W=["identf"], stream="c")

    stZ = contextlib.ExitStack()
    zb = sb(stZ, "zb", [128, D], BF16)
    zf = sb(stZ, "zf", [128, 8])
    op("dve", lambda e: e.memset(zb[:], 0.0), W=["zb"])
    op("dve", lambda e: e.memset(zf[:], 0.0), W=["zf"])
    for r0 in range(0, 8 * CAP, 128):
        dma("sp", hn_scr[r0:r0 + 128, :], zb[:], R=["zb"], W=["hn_scr"], stream="z")
        dma("sp", gt_scr[r0:r0 + 128, :], zf[:], R=["zf"], W=["gt_scr"], stream="z")
    phase_end()
    stZ.close()

    stM = contextlib.ExitStack()
    mixed = sb(stM, "mixed", [128, 16, TP], BF16)
    bc_scr = [nc.dram_tensor(f"bc_scr{i}", [128, 32 * 128], BF16, kind="Internal").ap() for i in range(4)]
    th2pi = sb(stM, "th2pi", [128, 32]); mag = sb(stM, "mag", [128, 32])
    car_re = sb(stM, "car_re", [128, 32]); car_im = sb(stM, "car_im", [128, 32])
    Sf = sb(stM, "Sf", [128, 8, 64]); Sb = sb(stM, "Sb", [128, 8, 64], BF16)
    xprev = sb(stM, "xprev", [128, 16, 1], BF16)
    ohacc = sb(stM, "ohacc", [128, 8])
    prm = {n: sb(stM, "p_" + n, [128, 8]) for n in ("s5d", "bglu", "w0", "a0", "k_k", "k_a", "r_k")}
    gmix = sb(stM, "gmix", [128, 16])
    w2b = sb(stM, "w2b", [64, 1024], BF16); a2b = sb(stM, "a2b", [64, 1024], BF16)
    g2b0 = sb(stM, "g2b0", [128, 1024], BF16); g2b1 = sb(stM, "g2b1", [32, 1024], BF16)
    for n, t in prm.items():
        dma("sp", t[:], I[n], W=["p_" + n], stream="c")
    dma("sp", gmix[:], I["gmix"], W=["gmix"], stream="c")
    dma("pool", w2b[:], I["w2"], W=["w2b"], stream="c")
    dma("pool", a2b[:], I["a2"], W=["a2b"], stream="c")
    dma("pool", g2b0[:], I["g2"][0:128, :], W=["g2b0"], stream="c")
    dma("pool", g2b1[:], I["g2"][128:160, :], W=["g2b1"], stream="c")
    for t_, kk_ in ((car_re, "car_re"), (car_im, "car_im"), (Sf, "Sf"), (Sb, "Sb"), (xprev, "xprev"), (ohacc, "ohacc")):
        op("dve", lambda e, t_=t_: e.memset(t_[:], 0.0), W=[kk_])

    w_in = I["w_in"].rearrange("(c p) n -> p c n", p=128)

    def s5_setup():
        st = contextlib.ExitStack()
        T = {n: sb(st, "s5_" + n, [128, 32]) for n in
             ("lr", "li", "ls", "dt", "lrd", "th", "q", "fr", "sn", "cs", "are", "aim", "nre", "den", "fre", "fim", "t1", "t2")}
        qi = sb(st, "s5_qi", [128, 32], I32)
        BT_re = sb(st, "BT_re", [128, 32, 128], BF16); BT_im = sb(st, "BT_im", [128, 32, 128], BF16)
        CT_re = sb(st, "CT_re", [128, 32, 128], BF16); CT_ni = sb(st, "CT_ni", [128, 32, 128], BF16)
        bre = sb(st, "s5_bre", [128, 1024]); bim = sb(st, "s5_bim", [128, 1024])
        o1 = sb(st, "s5_o1", [128, 128]); o2 = sb(st, "s5_o2", [128, 128], BF16); o3 = sb(st, "s5_o3", [128, 128], BF16)
        dma("sp", T["lr"][:], I["lam_re"], W=["lr"], stream="c")
        dma("sp", T["li"][:], I["lam_im"], W=["li"], stream="c")
        dma("sp", T["ls"][:], I["lstep"], W=["ls"], stream="c")
        A = lambda o, i_, f, **kw: op("act", lambda e: e.activation(out=T[o][:], in_=T[i_][:], func=f, **kw), R=[i_], W=[o])
        TT = lambda o, a, b, o_: op("dve", lambda e: e.tensor_tensor(out=T[o][:], in0=T[a][:], in1=T[b][:], op=o_), R=[a, b], W=[o])
        A("dt", "ls", AF.Exp)
        TT("lrd", "lr", "dt", ALU.mult)
        TT("th", "li", "dt", ALU.mult)
        op("act", lambda e: e.activation(out=mag[:], in_=T["lrd"][:], func=AF.Exp), R=["lrd"], W=["mag"])
        op("dve", lambda e: e.tensor_scalar(out=th2pi[:], in0=T["th"][:], scalar1=1.0 / TWO_PI, scalar2=None, op0=ALU.mult), R=["th"], W=["th2pi"])
        for nm, off in (("sn", 0.0), ("cs", 0.25)):
            op("dve", lambda e, off=off: e.tensor_scalar(out=T["q"][:], in0=th2pi[:], scalar1=off, scalar2=None, op0=ALU.add), R=["th2pi"], W=["q"])
            op("dve", lambda e: e.tensor_copy(out=qi[:], in_=T["q"][:]), R=["q"], W=["qi"])
            op("dve", lambda e: e.tensor_tensor(out=T["fr"][:], in0=T["q"][:], in1=qi[:], op=ALU.subtract), R=["q", "qi"], W=["fr"])
            op("act", lambda e, nm=nm: e.activation(out=T[nm][:], in_=T["fr"][:], func=AF.Sin, scale=TWO_PI), R=["fr"], W=[nm])
        op("dve", lambda e: e.tensor_tensor(out=T["are"][:], in0=mag[:], in1=T["cs"][:], op=ALU.mult), R=["mag", "cs"], W=["are"])
        op("dve", lambda e: e.tensor_tensor(out=T["aim"][:], in0=mag[:], in1=T["sn"][:], op=ALU.mult), R=["mag", "sn"], W=["aim"])
        op("dve", lambda e: e.tensor_scalar(out=T["nre"][:], in0=T["are"][:], scalar1=-1.0, scalar2=None, op0=ALU.add), R=["are"], W=["nre"])
        TT("t1", "lr", "lr", ALU.mult); TT("t2", "li", "li", ALU.mult); TT("den", "t1", "t2", ALU.add)
        op("dve", lambda e: e.reciprocal(out=T["den"][:], in_=T["den"][:]), R=["den"], W=["den"])
        TT("t1", "nre", "lr", ALU.mult); TT("t2", "aim", "li", ALU.mult); TT("fre", "t1", "t2", ALU.add); TT("fre", "fre", "den", ALU.mult)
        TT("t1", "aim", "lr", ALU.mult); TT("t2", "nre", "li", ALU.mult); TT("fim", "t1", "t2", ALU.subtract); TT("fim", "fim", "den", ALU.mult)
        for b8 in range(4):
            dma("sp", bre[:], I["Bx_re"][:, b8 * 1024:(b8 + 1) * 1024], W=["bre"], stream="c")
            dma("sp", bim[:], I["Bx_im"][:, b8 * 1024:(b8 + 1) * 1024], W=["bim"], stream="c")
            for j in range(8):
                b = b8 * 8 + j
                sl = slice(j * 128, (j + 1) * 128)
                op("dve", lambda e, sl=sl, b=b: e.tensor_scalar(out=o1[:], in0=bim[:, sl], scalar1=T["fim"][:, b:b + 1], scalar2=None, op0=ALU.mult), R=["bim", "fim"], W=["o1"])
                op("dve", lambda e, sl=sl, b=b: e.scalar_tensor_tensor(out=o2[:], in0=bre[:, sl], scalar=T["fre"][:, b:b + 1], in1=o1[:], op0=ALU.mult, op1=ALU.subtract), R=["bre", "fre", "o1"], W=["o2"])
                op("dve", lambda e, sl=sl, b=b: e.tensor_scalar(out=o1[:], in0=bre[:, sl], scalar1=T["fim"][:, b:b + 1], scalar2=None, op0=ALU.mult), R=["bre", "fim", "o2"], W=["o1"])
                op("dve", lambda e, sl=sl, b=b: e.scalar_tensor_tensor(out=o3[:], in0=bim[:, sl], scalar=T["fre"][:, b:b + 1], in1=o1[:], op0=ALU.mult, op1=ALU.add), R=["bim", "fre", "o1"], W=["o3"])
                for src, srck, dst, dstk in ((o2, "o2", BT_re, "BT_re"), (o3, "o3", BT_im, "BT_im")):
                    pt, pk = ps()
                    op("pe", lambda e, src=src, pt=pt: e.matmul(pt[:, 0:128], src[:], ident[:], start=True, stop=True), R=[srck, "identb"], W=[pk])
                    op("act", lambda e, dst=dst, pt=pt, b=b: e.activation(out=dst[:, b, :], in_=pt[:, 0:128], func=AF.Copy), R=[pk], W=[dstk])
            dma("sp", bre[:], I["Cx_re"][:, b8 * 1024:(b8 + 1) * 1024], W=["bre"], stream="c")
            dma("sp", bim[:], I["Cx_im"][:, b8 * 1024:(b8 + 1) * 1024], W=["bim"], stream="c")
            op("act", lambda e, b8=b8: e.activation(out=CT_re[:, b8 * 8:(b8 + 1) * 8, :], in_=bre[:].rearrange("p (a b) -> p a b", b=128), func=AF.Copy), R=["bre"], W=["CT_re"])
            op("act", lambda e, b8=b8: e.activation(out=CT_ni[:, b8 * 8:(b8 + 1) * 8, :], in_=bim[:].rearrange("p (a b) -> p a b", b=128), func=AF.Identity, scale=-1.0), R=["bim"], W=["CT_ni"])
        for i_, (t_, k_) in enumerate(((BT_re, "BT_re"), (BT_im, "BT_im"), (CT_re, "CT_re"), (CT_ni, "CT_ni"))):
            dma("sp", bc_scr[i_], t_[:].rearrange("p a b -> p (a b)"), R=[k_], W=["bc_scr"], stream="c")
        phase_end()
        st.close()

    s5_setup()

    def mixer_pass(p):
        full = p >= 2
        stA = contextlib.ExitStack()
        xh = sb(stA, "xh", [128, 16, TP + 1], BF16)
        wst = sb(stA, "wst", [128, 16, 128])
        mut = sb(stA, "mut", [128, 128]); omut = sb(stA, "omut", [128, 128])
        stX = contextlib.ExitStack()
        xst = [sb(stX, f"xst{i}", [128, NT]) for i in range(2)]
        sqt = [sb(stX, f"sq{i}", [128, NT], BF16) for i in range(2)]
        rstd = sb(stX, "rstd", [128, NT])
        onesb = sb(stX, "onesb", [128, 128], BF16)
        wt = {}
        op("dve", lambda e: e.memset(onesb[:], 1.0), W=["onesb"])
        op("act", lambda e: e.activation(out=xh[:, :, 0:1], in_=xprev[:], func=AF.Copy), R=["xprev"], W=["xh"])
        for t in range(TP // NT):
            t0 = p * TP + t * NT
            pss, pk = ps()
            for c in range(16):
                xs, xk = xst[c % 2], f"xst{c % 2}"
                dma("sp", xs[:], I["xT"][c * 128:(c + 1) * 128, t0:t0 + NT], W=[xk], stream="x")
                op("act", lambda e, c=c, xs=xs: e.activation(out=sqt[c % 2][:], in_=xs[:], func=AF.Square), R=[xk], W=[f"sq{c % 2}"])
                op("pe", lambda e, c=c: e.matmul(pss[:], onesb[:], sqt[c % 2][:], start=(c == 0), stop=(c == 15)), R=[f"sq{c % 2}", "onesb"], W=[pk])
            op("act", lambda e: e.activation(out=rstd[:], in_=pss[:], func=AF.Sqrt, scale=1.0 / D, bias=1e-6), R=[pk], W=["rstd"])
            op("dve", lambda e: e.reciprocal(out=rstd[:], in_=rstd[:]), R=["rstd"], W=["rstd"])
            for c in range(16):
                xs, xk = xst[c % 2], f"xst{c % 2}"
                dma("sp", xs[:], I["xT"][c * 128:(c + 1) * 128, t0:t0 + NT], W=[xk], stream="x")
                op("dve", lambda e, c=c, t=t, xs=xs: e.scalar_tensor_tensor(
                    out=xh[:, c, 1 + t * NT:1 + (t + 1) * NT], in0=xs[:], scalar=gmix[:, c:c + 1], in1=rstd[:],
                    op0=ALU.mult, op1=ALU.mult), R=[xk, "rstd", "gmix"], W=["xh"])
        op("act", lambda e: e.activation(out=xprev[:], in_=xh[:, :, TP:TP + 1], func=AF.Copy), R=["xh"], W=["xprev"])
        if p == 2:
            tap("xh", xh[:].rearrange("p a b -> p (a b)"), ["xh"])
        phase_end()
        stX.close()

        def load_w(col0, ncols, shifted, slot):
            w1, w0 = wt[(slot, 1)], wt[(slot, 0)]
            dma("sp", wst[:, :, :ncols], w_in[:, :, col0:col0 + ncols], W=["wst"], stream="w")
            if not shifted:
                op("act", lambda e: e.activation(out=w1[:, :, :ncols], in_=wst[:, :, :ncols], func=AF.Copy), R=["wst"], W=[f"w1_{slot}"])
                return (w1, f"w1_{slot}"), None
            m0 = col0 - 1024
            dma("sp", mut[:, :ncols], I["mu_bc"][:, m0:m0 + ncols], W=["mut"], stream="w")
            op("pool", lambda e: e.tensor_scalar(out=omut[:, :ncols], in0=mut[:, :ncols], scalar1=-1.0, scalar2=1.0, op0=ALU.mult, op1=ALU.add), R=["mut"], W=["omut"])
            for c in range(16):
                op("pool", lambda e, c=c: e.tensor_tensor(out=w1[:, c, :ncols], in0=wst[:, c, :ncols], in1=omut[:, :ncols], op=ALU.mult), R=["wst", "omut"], W=[f"w1_{slot}"])
                op("pool", lambda e, c=c: e.tensor_tensor(out=w0[:, c, :ncols], in0=wst[:, c, :ncols], in1=mut[:, :ncols], op=ALU.mult), R=["wst", "mut"], W=[f"w0_{slot}"])
            return (w1, f"w1_{slot}"), (w0, f"w0_{slot}")

        def proj_fm(pst, pk, W1, W0, ncols, t):
            n = 16 * (2 if W0 else 1)
            i = 0
            for c in range(16):
                op("pe", lambda e, c=c, i=i: e.matmul(pst[0:ncols, :], W1[0][:, c, 0:ncols], xh[:, c, 1 + t * NT:1 + (t + 1) * NT], start=(i == 0), stop=(i == n - 1)), R=["xh", W1[1]], W=[pk])
                i += 1
                if W0:
                    op("pe", lambda e, c=c, i=i: e.matmul(pst[0:ncols, :], W0[0][:, c, 0:ncols], xh[:, c, t * NT:(t + 1) * NT], start=False, stop=(i == n - 1)), R=["xh", W0[1]], W=[pk])
                    i += 1

        def s5_pass():
            st = contextlib.ExitStack()
            F = {n: sb(st, "f_" + n, [128, NT]) for n in ("uf", "q", "cn", "sn", "zr", "zi", "t1", "rr", "ri", "magt", "y1", "x2")}
            qi = sb(st, "f_qi", [128, NT], I32)
            ub = sb(st, "f_ub", [128, NT], BF16); sre = sb(st, "f_sre", [128, NT], BF16); sim = sb(st, "f_sim", [128, NT], BF16)
            tidx = sb(st, "tidx", [128, NT]); onesf = sb(st, "onesf", [128, NT])
            BT_re = sb(st, "BT_re", [128, 32, 128], BF16); BT_im = sb(st, "BT_im", [128, 32, 128], BF16)
            CT_re = sb(st, "CT_re", [128, 32, 128], BF16); CT_ni = sb(st, "CT_ni", [128, 32, 128], BF16)
            for i_, (t_, k_) in enumerate(((BT_re, "BT_re"), (BT_im, "BT_im"), (CT_re, "CT_re"), (CT_ni, "CT_ni"))):
                dma("sp", t_[:].rearrange("p a b -> p (a b)"), bc_scr[i_], R=["bc_scr"], W=[k_], stream="c")
            wt.update({(v, j): sb(st, f"wt{v}{j}", [128, 16, 128], BF16) for v in range(1) for j in range(2)})
            qb0 = sb(st, "qb0", [128, 32]); qb1 = sb(st, "qb1", [128, 32])
            dma("sp", tidx[:], I["tidx"], W=["tidx"], stream="c")
            op("dve", lambda e: e.memset(onesf[:], 1.0), W=["onesf"])
            if full:
                yg = sb(st, "yg", [128, 8, TP], BF16)
                wg = sb(st, "wglu", [128, 8, 128], BF16)
            TT = lambda eng, o, a, b, o_: op(eng, lambda e: e.tensor_tensor(out=o[0][:], in0=a[0][:], in1=b[0][:], op=o_), R=[a[1], b[1]], W=[o[1]])
            f = lambda n: (F[n], "f_" + n)
            for gb in range(8):
                W1, _ = load_w(gb * 128, 128, False, 0)
                if p == 2 and gb == 0:
                    tap("w1_0", W1[0][:].rearrange("p a b -> p (a b)"), [W1[1]])
                for t in range(TP // NT):
                    toff = float(p * TP + t * NT)
                    op("dve", lambda e, toff=toff: e.tensor_scalar(out=qb0[:], in0=th2pi[:], scalar1=toff, scalar2=None, op0=ALU.mult), R=["th2pi"], W=["qb"])
                    op("dve", lambda e, toff=toff: e.tensor_scalar(out=qb1[:], in0=th2pi[:], scalar1=toff, scalar2=0.25, op0=ALU.mult, op1=ALU.add), R=["th2pi"], W=["qb"])
                    pu, pku = ps()
                    proj_fm(pu, pku, W1, None, 128, t)
                    op("act", lambda e: e.activation(out=ub[:], in_=pu[:], func=AF.Copy), R=[pku], W=["f_ub"])
                    if full:
                        op("act", lambda e: e.activation(out=F["uf"][:], in_=pu[:], func=AF.Copy), R=[pku], W=["f_uf"])
                        if p == 2 and gb == 0 and t == 0:
                            tap("u0", F["uf"][:], ["f_uf"])
                        py, pky = ps(pin=True)
                    for bl in range(4):
                        b = gb * 4 + bl
                        pr, pkr = ps()
                        pi_, pki = ps()
                        op("pe", lambda e, b=b, pr=pr: e.matmul(pr[:], BT_re[:, b, :], ub[:], start=True, stop=True), R=["BT_re", "f_ub"], W=[pkr])
                        op("pe", lambda e, b=b, pi_=pi_: e.matmul(pi_[:], BT_im[:, b, :], ub[:], start=True, stop=True), R=["BT_im", "f_ub"], W=[pki])
                        for nm, qbt in (("sn", qb0), ("cn", qb1)):
                            op("act", lambda e, b=b, qbt=qbt: e.activation(out=F["q"][:], in_=tidx[:], func=AF.Identity, scale=th2pi[:, b:b + 1], bias=qbt[:, b:b + 1]), R=["tidx", "th2pi", "qb"], W=["f_q"])
                            op("pool", lambda e: e.tensor_copy(out=qi[:], in_=F["q"][:]), R=["f_q"], W=["f_qi"])
                            op("pool", lambda e: e.tensor_tensor(out=F["q"][:], in0=F["q"][:], in1=qi[:], op=ALU.subtract), R=["f_q", "f_qi"], W=["f_q"])
                            op("act", lambda e, nm=nm: e.activation(out=F[nm][:], in_=F["q"][:], func=AF.Sin, scale=TWO_PI), R=["f_q"], W=["f_" + nm])
                        P = lambda t_, k_: (t_, k_)
                        TT("dve", f("zr"), P(pr, pkr), f("cn"), ALU.mult)
                        TT("dve", f("t1"), P(pi_, pki), f("sn"), ALU.mult)
                        TT("dve", f("zr"), f("zr"), f("t1"), ALU.add)
                        TT("dve", f("zi"), P(pi_, pki), f("cn"), ALU.mult)
                        TT("dve", f("t1"), P(pr, pkr), f("sn"), ALU.mult)
                        TT("dve", f("zi"), f("zi"), f("t1"), ALU.subtract)
                        op("act", lambda e, b=b: e.activation(out=F["magt"][:], in_=onesf[:], func=AF.Identity, scale=mag[:, b:b + 1]), R=["onesf", "mag"], W=["f_magt"])
                        op("dve", lambda e, b=b: e.tensor_tensor_scan(out=F["rr"][:], data0=F["magt"][:], data1=F["zr"][:], initial=car_re[:, b:b + 1], op0=ALU.mult, op1=ALU.add), R=["f_magt", "f_zr", "car_re"], W=["f_rr"])
                        op("dve", lambda e, b=b: e.tensor_tensor_scan(out=F["ri"][:], data0=F["magt"][:], data1=F["zi"][:], initial=car_im[:, b:b + 1], op0=ALU.mult, op1=ALU.add), R=["f_magt", "f_zi", "car_im"], W=["f_ri"])
                        op("act", lambda e, b=b: e.activation(out=car_re[:, b:b + 1], in_=F["rr"][:, NT - 1:NT], func=AF.Copy), R=["f_rr"], W=["car_re"])
                        op("act", lambda e, b=b: e.activation(out=car_im[:, b:b + 1], in_=F["ri"][:, NT - 1:NT], func=AF.Copy), R=["f_ri"], W=["car_im"])
                        if full:
                            TT("dve", f("zr"), f("cn"), f("rr"), ALU.mult)
                            TT("dve", f("t1"), f("sn"), f("ri"), ALU.mult)
                            TT("dve", (sre, "f_sre"), f("zr"), f("t1"), ALU.subtract)
                            TT("dve", f("zi"), f("cn"), f("ri"), ALU.mult)
                            TT("dve", f("t1"), f("sn"), f("rr"), ALU.mult)
                            TT("dve", (sim, "f_sim"), f("zi"), f("t1"), ALU.add)
                            if p == 2 and b == 0 and t == 0:
                                tap("sre0", sre[:], ["f_sre"]); tap("rr0", F["rr"][:], ["f_rr"]); tap("cn0", F["cn"][:], ["f_cn"]); tap("sn0", F["sn"][:], ["f_sn"])
                            op("pe", lambda e, b=b, bl=bl, py=py: e.matmul(py[:], CT_re[:, b, :], sre[:], start=(bl == 0), stop=False), R=["CT_re", "f_sre"], W=[pky])
                            op("pe", lambda e, b=b, bl=bl, py=py: e.matmul(py[:], CT_ni[:, b, :], sim[:], start=False, stop=(bl == 3)), R=["CT_ni", "f_sim"], W=[pky])
                    if full:
                        sd = prm["s5d"]
                        op("dve", lambda e, gb=gb, py=py: e.scalar_tensor_tensor(out=F["y1"][:], in0=F["uf"][:], scalar=sd[:, gb:gb + 1], in1=py[:], op0=ALU.mult, op1=ALU.add), R=["f_uf", "p_s5d", pky], W=["f_y1"])
                        unpin(pky)
                        if p == 2 and gb == 0 and t == 0:
                            tap("y1_0", F["y1"][:], ["f_y1"])
                        op("act", lambda e: e.activation(out=F["x2"][:], in_=F["y1"][:], func=AF.Square), R=["f_y1"], W=["f_x2"])
                        op("dve", lambda e: e.tensor_scalar(out=F["x2"][:], in0=F["x2"][:], scalar1=0.044715, scalar2=1.0, op0=ALU.mult, op1=ALU.add), R=["f_x2"], W=["f_x2"])
                        TT("dve", f("x2"), f("x2"), f("y1"), ALU.mult)
                        op("act", lambda e: e.activation(out=F["x2"][:], in_=F["x2"][:], func=AF.Sigmoid, scale=1.5957691216057308), R=["f_x2"], W=["f_x2"])
                        op("dve", lambda e, gb=gb, t=t: e.tensor_tensor(out=yg[:, gb, t * NT:(t + 1) * NT], in0=F["y1"][:], in1=F["x2"][:], op=ALU.mult), R=["f_y1", "f_x2"], W=["yg"])
            if full:
                wglu = I["w_glu"].rearrange("(c p) n -> p c n", p=128)
                for oc in range(8):
                    dma("pool", wg[:], wglu[:, :, oc * 128:(oc + 1) * 128], W=["wglu"], stream="g")
                    for t in range(TP // NT):
                        pg, pkg = ps()
                        for kc in range(8):
                            op("pe", lambda e, kc=kc, t=t, pg=pg: e.matmul(pg[:], wg[:, kc, :], yg[:, kc, t * NT:(t + 1) * NT], start=(kc == 0), stop=(kc == 7)), R=["wglu", "yg"], W=[pkg])
                        op("act", lambda e, oc=oc, pg=pg: e.activation(out=F["x2"][:], in_=pg[:], func=AF.Sigmoid, bias=prm["bglu"][:, oc:oc + 1]), R=[pkg, "p_bglu"], W=["f_x2"])
                        op("dve", lambda e, oc=oc, t=t: e.tensor_tensor(out=mixed[:, oc, t * NT:(t + 1) * NT], in0=yg[:, oc, t * NT:(t + 1) * NT], in1=F["x2"][:], op=ALU.mult), R=["yg", "f_x2"], W=["mixed"])
            phase_end()
            st.close()

        k_ = dict(load_w=load_w, proj_fm=proj_fm, xh=xh, stA=stA, wt=wt)
        return k_, s5_pass

    def rwkv_pass(p, kA):
        full = p >= 2
        load_w, proj_fm, xh, wt = kA["load_w"], kA["proj_fm"], kA["xh"], kA["wt"]
        st = contextlib.ExitStack()
        wt.update({(v, j): sb(st, f"wt{v}{j}", [128, 16, 128], BF16) for v in range(3) for j in range(2)})
        mLT = sb(st, "mLT", [64, 512], BF16); mLE = sb(st, "mLE", [64, 512], BF16); mGT = sb(st, "mGT", [64, 512], BF16)
        idrep = sb(st, "idrep", [64, 512], BF16)
        rstm = sb(st, "rstm", [128, NT]); bones = sb(st, "bones", [128, 128], BF16); hsel = sb(st, "hsel", [128, 2], BF16)
        for t_, n_ in ((mLT, "maskLT"), (mLE, "maskLE"), (mGT, "maskGT"), (idrep, "identrep"), (bones, "bones"), (hsel, "hsel")):
            dma("pool", t_[:], I[n_], W=["c_" + n_], stream="c")
        dma("sp", rstm[:], I["rstmask"], W=["rstm"], stream="c")
        CK = ["c_maskLT", "c_maskLE", "c_maskGT", "c_identrep"]
        txw = sb(st, "txw", [64, TP], BF16); xab = sb(st, "xab", [64, TP], BF16)
        sgx0 = sb(st, "sgx0", [128, TP], BF16); sgx1 = sb(st, "sgx1", [32, TP], BF16)
        lnw = sb(st, "lnw", [64, 128]); lnb = sb(st, "lnb", [64, 128])
        Fm = {n: sb(st, "r_" + n, [128, NT]) for n in ("lw", "cw", "ag", "kk", "rn", "kmod", "ep", "en", "ex")}
        ssq = sb(st, "r_ssq", [128, NT], BF16)
        ar = sb(st, "r_ar", [128, 8, 128], BF16)
        btf = sb(st, "r_bt", [128, NT], BF16); ktf = sb(st, "r_kt", [128, NT], BF16)
        rkr = sb(st, "r_rkr", [128, NT], BF16); vf = sb(st, "r_vf", [128, NT], BF16)
        vtok = sb(st, "r_vtok", [64, 8, 128], BF16); bttok = sb(st, "r_bttok", [64, 8, 128], BF16); kttok = sb(st, "r_kttok", [64, 8, 128], BF16)
        gtok = sb(st, "r_gtok", [64, 8, 128], BF16); bon = sb(st, "r_bon", [64, 16])
        AM = {(n, h): sb(st, f"r_{n}{h}", [64, 8, 64], BF16) for n in ("Aab", "Abr", "Aak", "Akr", "Minv") for h in range(2)}
        CH = {n: sb(st, "r_c" + n, [64, 8, 64], BF16) for n in ("A0", "A1", "T0", "T1", "P0")}
        xsb = sb(st, "r_xsb", [64, 128], BF16); usb = sb(st, "r_usb", [64, 128], BF16)
        ytok = sb(st, "r_ytok", [64, 8, 128]); ysq = sb(st, "r_ysq", [64, 8, 128]); otok = sb(st, "r_otok", [64, 8, 128], BF16)
        S1 = sb(st, "r_S1", [128, 64]); yn = sb(st, "r_yn", [64, 128])
        sm = {n: sb(st, "r_s" + n, [64, 16]) for n in ("s1", "s2", "mn", "vr", "rs", "nm")}

        for col0, ncols, dst, dk, fn in ((4096, 64, txw, "txw", AF.Tanh), (4160, 64, xab, "xab", AF.Identity),
                                         (4224, 128, sgx0, "sgx0", AF.Sigmoid), (4352, 32, sgx1, "sgx1", AF.Sigmoid)):
            if not full and dk.startswith("sgx"):
                continue
            W1, W0 = load_w(col0, ncols, True, 0)
            for t in range(TP // NT):
                pt, pk = ps()
                proj_fm(pt, pk, W1, W0, ncols, t)
                op("act", lambda e, dst=dst, ncols=ncols, t=t, pt=pt, fn=fn: e.activation(out=dst[0:ncols, t * NT:(t + 1) * NT], in_=pt[0:ncols, :], func=fn), R=[pk], W=[dk])

        fk = lambda n: "r_" + n
        LV = DEBUG.get("lv", 9)
        for hp in range(DEBUG.get("nhp", 8)):
            if LV < 2:
                break
            hc = slice(hp * 128, (hp + 1) * 128)
            Wr = load_w(1024 + hp * 128, 128, True, 0)
            Wk = load_w(2048 + hp * 128, 128, True, 1)
            Wv = load_w(3072 + hp * 128, 128, True, 2)
            if full:
                dma("sp", lnw[:], I["lnw_bc"][:, 1024 * 0 + hp * 128:(hp + 1) * 128], W=["lnw"], stream="c")
                dma("sp", lnb[:], I["lnb_bc"][:, hp * 128:(hp + 1) * 128], W=["lnb"], stream="c")
            for t in range(TP // NT):
                ts_ = slice(t * NT, (t + 1) * NT)
                pr, pkr = ps(); proj_fm(pr, pkr, Wr[0], Wr[1], 128, t)
                pkk, pkkk = ps(); proj_fm(pkk, pkkk, Wk[0], Wk[1], 128, t)
                pv, pkv = ps(); proj_fm(pv, pkv, Wv[0], Wv[1], 128, t)
                op("act", lambda e, pv=pv: e.activation(out=vf[:], in_=pv[:], func=AF.Copy), R=[pkv], W=["r_vf"])
                pz, pkz = ps()
                op("pe", lambda e, pz=pz, hc=hc, ts_=ts_: e.matmul(pz[:], w2b[:, hc], txw[:, ts_], start=True, stop=True), R=["w2b", "txw"], W=[pkz])
                op("act", lambda e, pz=pz, hp=hp: e.activation(out=Fm["lw"][:], in_=pz[:], func=AF.Sigmoid, bias=prm["w0"][:, hp:hp + 1]), R=[pkz, "p_w0"], W=[fk("lw")])
                op("dve", lambda e: e.tensor_scalar(out=Fm["lw"][:], in0=Fm["lw"][:], scalar1=-0.6065306597126334, scalar2=None, op0=ALU.mult), R=[fk("lw")], W=[fk("lw")])
                op("dve", lambda e: e.tensor_tensor_scan(out=Fm["cw"][:], data0=rstm[:], data1=Fm["lw"][:], initial=0.0, op0=ALU.mult, op1=ALU.add), R=["rstm", fk("lw")], W=[fk("cw")])
                pa, pka = ps()
                op("pe", lambda e, pa=pa, hc=hc, ts_=ts_: e.matmul(pa[:], a2b[:, hc], xab[:, ts_], start=True, stop=True), R=["a2b", "xab"], W=[pka])
                op("act", lambda e, pa=pa, hp=hp: e.activation(out=Fm["ag"][:], in_=pa[:], func=AF.Sigmoid, bias=prm["a0"][:, hp:hp + 1]), R=[pka, "p_a0"], W=[fk("ag")])
                op("act", lambda e, pkk=pkk, hp=hp: e.activation(out=Fm["kk"][:], in_=pkk[:], func=AF.Identity, scale=prm["k_k"][:, hp:hp + 1]), R=[pkkk, "p_k_k"], W=[fk("kk")])
                op("act", lambda e: e.activation(out=ssq[:], in_=Fm["kk"][:], func=AF.Square), R=[fk("kk")], W=["r_ssq"])
                pss, pks = ps()
                op("pe", lambda e, pss=pss: e.matmul(pss[:], bones[:], ssq[:], start=True, stop=True), R=["c_bones", "r_ssq"], W=[pks])
                op("act", lambda e, pss=pss: e.activation(out=Fm["rn"][:], in_=pss[:], func=AF.Sqrt), R=[pks], W=[fk("rn")])
                op("dve", lambda e: e.tensor_scalar(out=Fm["rn"][:], in0=Fm["rn"][:], scalar1=1e-12, scalar2=None, op0=ALU.max), R=[fk("rn")], W=[fk("rn")])
                op("dve", lambda e: e.reciprocal(out=Fm["rn"][:], in_=Fm["rn"][:]), R=[fk("rn")], W=[fk("rn")])
                op("dve", lambda e: e.tensor_tensor(out=Fm["kk"][:], in0=Fm["kk"][:], in1=Fm["rn"][:], op=ALU.mult), R=[fk("kk"), fk("rn")], W=[fk("kk")])
                op("dve", lambda e, hp=hp: e.tensor_scalar(out=Fm["rn"][:], in0=Fm["ag"][:], scalar1=-1.0, scalar2=prm["k_a"][:, hp:hp + 1], op0=ALU.add, op1=ALU.mult), R=[fk("ag"), "p_k_a", fk("kk")], W=[fk("rn")])
                op("dve", lambda e, pkk=pkk: e.scalar_tensor_tensor(out=Fm["kmod"][:], in0=Fm["rn"][:], scalar=1.0, in1=pkk[:], op0=ALU.add, op1=ALU.mult), R=[fk("rn"), pkkk], W=[fk("kmod")])
                op("act", lambda e: e.activation(out=Fm["ep"][:], in_=Fm["cw"][:], func=AF.Exp), R=[fk("cw")], W=[fk("ep")])
                op("act", lambda e: e.activation(out=Fm["en"][:], in_=Fm["cw"][:], func=AF.Exp, scale=-1.0), R=[fk("cw")], W=[fk("en")])
                op("dve", lambda e: e.tensor_tensor(out=Fm["ex"][:], in0=Fm["cw"][:], in1=Fm["lw"][:], op=ALU.subtract), R=[fk("cw"), fk("lw")], W=[fk("ex")])
                op("act", lambda e: e.activation(out=Fm["ex"][:], in_=Fm["ex"][:], func=AF.Exp), R=[fk("ex")], W=[fk("ex")])
                v3 = lambda a: a[:].rearrange("p (c t) -> p c t", t=64)
                op("dve", lambda e, pr=pr: e.tensor_tensor(out=ar[:, :, 64:128], in0=v3(pr), in1=v3(Fm["ep"]), op=ALU.mult), R=[pkr, fk("ep")], W=["r_ar"])
                op("dve", lambda e: e.scalar_tensor_tensor(out=ar[:, :, 0:64], in0=v3(Fm["kk"]), scalar=-1.0, in1=v3(Fm["ex"]), op0=ALU.mult, op1=ALU.mult), R=[fk("kk"), fk("ex")], W=["r_ar"])
                op("dve", lambda e: e.tensor_tensor(out=Fm["rn"][:], in0=Fm["kk"][:], in1=Fm["ag"][:], op=ALU.mult), R=[fk("kk"), fk("ag"), fk("kmod")], W=[fk("rn")])
                op("dve", lambda e: e.tensor_tensor(out=btf[:], in0=Fm["rn"][:], in1=Fm["en"][:], op=ALU.mult), R=[fk("rn"), fk("en")], W=["r_bt"])
                op("dve", lambda e: e.tensor_tensor(out=ktf[:], in0=Fm["kmod"][:], in1=Fm["en"][:], op=ALU.mult), R=[fk("kmod"), fk("en")], W=["r_kt"])
                if full:
                    op("dve", lambda e, pr=pr, hp=hp: e.scalar_tensor_tensor(out=rkr[:], in0=Fm["kmod"][:], scalar=prm["r_k"][:, hp:hp + 1], in1=pr[:], op0=ALU.mult, op1=ALU.mult), R=[fk("kmod"), "p_r_k", pkr], W=["r_rkr"])
                if LV < 3:
                    continue
                def tr(src, srck, dst, dstk):
                    for half in range(2):
                        pt, pk = ps()
                        for c4 in range(4):
                            c = half * 4 + c4
                            op("pe", lambda e, c=c, c4=c4, pt=pt: e.matmul(pt[0:64, c4 * 128:(c4 + 1) * 128], src[:, c * 64:(c + 1) * 64], ident[:], start=True, stop=True), R=[srck, "identb"], W=[pk])
                        op("act", lambda e, half=half, pt=pt: e.activation(out=dst[:, half * 4:(half + 1) * 4, :], in_=pt[0:64, :].rearrange("p (c t) -> p c t", t=128), func=AF.Copy), R=[pk], W=[dstk])
                tr(vf, "r_vf", vtok, "r_vtok"); tr(btf, "r_bt", bttok, "r_bttok"); tr(ktf, "r_kt", kttok, "r_kttok")
                if full:
                    for half in range(2):
                        pt, pk = ps()
                        for c4 in range(4):
                            c = half * 4 + c4
                            tk = slice(t * NT + c * 64, t * NT + (c + 1) * 64)
                            op("pe", lambda e, c4=c4, pt=pt, tk=tk: e.matmul(pt[0:64, c4 * 128:(c4 + 1) * 128], sgx0[:, tk], g2b0[:, hc], start=True, stop=False), R=["sgx0", "g2b0"], W=[pk])
                            op("pe", lambda e, c4=c4, pt=pt, tk=tk: e.matmul(pt[0:64, c4 * 128:(c4 + 1) * 128], sgx1[:, tk], g2b1[:, hc], start=False, stop=True), R=["sgx1", "g2b1"], W=[pk])
                        op("act", lambda e, half=half, pt=pt: e.activation(out=gtok[:, half * 4:(half + 1) * 4, :], in_=pt[0:64, :].rearrange("p (c t) -> p c t", t=128), func=AF.Copy), R=[pk], W=["r_gtok"])
                    pt, pk = ps()
                    for c in range(8):
                        op("pe", lambda e, c=c, pt=pt: e.matmul(pt[0:64, c * 2:(c + 1) * 2], rkr[:, c * 64:(c + 1) * 64], hsel[:], start=True, stop=True), R=["r_rkr", "c_hsel"], W=[pk])
                    op("act", lambda e, pt=pt: e.activation(out=bon[:], in_=pt[0:64, 0:16], func=AF.Copy), R=[pk], W=["r_bon"])
                if LV < 4:
                    continue
                for hl in range(2):
                    pb = slice(hl * 64, (hl + 1) * 64)
                    for src, n1, n2 in ((btf, "Aab", "Abr"), (ktf, "Aak", "Akr")):
                        for half in range(2):
                            pt, pk = ps()
                            for c4 in range(4):
                                c = half * 4 + c4
                                op("pe", lambda e, c=c, c4=c4, pt=pt, src=src: e.matmul(pt[0:64, c4 * 128:(c4 + 1) * 128], src[pb, c * 64:(c + 1) * 64], ar[pb, c, :], start=True, stop=True), R=["r_bt", "r_kt", "r_ar"], W=[pk])
                            pv4 = pt[0:64, :].rearrange("p (c t) -> p c t", t=128)
                            op("dve", lambda e, half=half, pv4=pv4, n1=n1: e.tensor_tensor(out=AM[(n1, hl)][:, half * 4:(half + 1) * 4, :], in0=pv4[:, :, 0:64], in1=mLT[:].rearrange("p (c t) -> p c t", t=64)[:, 0:4, :], op=ALU.mult), R=[pk] + CK, W=[f"{n1}{hl}"])
                            op("dve", lambda e, half=half, pv4=pv4, n2=n2: e.tensor_tensor(out=AM[(n2, hl)][:, half * 4:(half + 1) * 4, :], in0=pv4[:, :, 64:128], in1=mLE[:].rearrange("p (c t) -> p c t", t=64)[:, 0:4, :], op=ALU.mult), R=[pk] + CK, W=[f"{n2}{hl}"])
                    pt, pk = ps()
                    for c in range(8):
                        op("pe", lambda e, c=c, pt=pt: e.matmul(pt[0:64, c * 64:(c + 1) * 64], ar[pb, c, 0:64], btf[pb, c * 64:(c + 1) * 64], start=True, stop=True), R=["r_ar", "r_bt"], W=[pk])
                    op("dve", lambda e, pt=pt: e.tensor_tensor(out=CH["T0"][:].rearrange("p c t -> p (c t)"), in0=pt[0:64, :], in1=mGT[:], op=ALU.mult), R=[pk] + CK, W=["cT0"])
                    Acur, Ak = AM[("Aab", hl)], f"Aab{hl}"
                    Tcur, Tk = CH["T0"], "cT0"
                    Pcur, Pk = CH["P0"], "cP0"
                    op("dve", lambda e, Acur=Acur: e.tensor_tensor(out=CH["P0"][:].rearrange("p c t -> p (c t)"), in0=Acur[:].rearrange("p c t -> p (c t)"), in1=idrep[:], op=ALU.add), R=[Ak] + CK, W=["cP0"])
                    for j in range(1, 6):
                        An, Ank = CH[f"A{j % 2}"], f"cA{j % 2}"
                        Tn, Tnk = CH[f"T{j % 2}"], f"cT{j % 2}"
                        if j < 5:
                            pt, pk = ps()
                            for c in range(8):
                                op("pe", lambda e, c=c, pt=pt, Tcur=Tcur, Acur=Acur: e.matmul(pt[0:64, c * 64:(c + 1) * 64], Tcur[:, c, :], Acur[:, c, :], start=True, stop=True), R=[Tk, Ak], W=[pk])
                            op("act", lambda e, pt=pt, An=An: e.activation(out=An[:].rearrange("p c t -> p (c t)"), in_=pt[0:64, :], func=AF.Copy), R=[pk], W=[Ank])
                        pt2, pk2 = ps()
                        for c in range(8):
                            op("pe", lambda e, c=c, pt2=pt2, Tcur=Tcur, Acur=Acur: e.matmul(pt2[0:64, c * 64:(c + 1) * 64], Acur[:, c, :], Tcur[:, c, :], start=True, stop=True), R=[Tk, Ak], W=[pk2])
                        op("act", lambda e, pt2=pt2, Tn=Tn: e.activation(out=Tn[:].rearrange("p c t -> p (c t)"), in_=pt2[0:64, :], func=AF.Copy), R=[pk2], W=[Tnk])
                        pt3, pk3 = ps()
                        for c in range(8):
                            op("pe", lambda e, c=c, pt3=pt3, Tn=Tn, Pcur=Pcur: e.matmul(pt3[0:64, c * 64:(c + 1) * 64], Tn[:, c, :], Pcur[:, c, :], start=True, stop=True), R=[Tnk, Pk], W=[pk3])
                        Pn, Pnk = (AM[("Minv", hl)], f"Minv{hl}") if j % 2 == 1 else (CH["P0"], "cP0")
                        op("dve", lambda e, pt3=pt3, Pn=Pn, Pcur=Pcur: e.tensor_tensor(out=Pn[:].rearrange("p c t -> p (c t)"), in0=pt3[0:64, :], in1=Pcur[:].rearrange("p c t -> p (c t)"), op=ALU.add), R=[pk3, Pk], W=[Pnk])
                        Acur, Ak, Tcur, Tk, Pcur, Pk = An, Ank, Tn, Tnk, Pn, Pnk
                if LV < 5:
                    continue
                for c in range(8):
                    px, pkx = ps()
                    for hl in range(2):
                        pb = slice(hl * 64, (hl + 1) * 64)
                        op("pe", lambda e, hl=hl, pb=pb, c=c, px=px: e.matmul(px[0:64, hl * 64:(hl + 1) * 64], ar[pb, c, 0:64], Sb[pb, hp, :], start=True, stop=False), R=["r_ar", "Sb"], W=[pkx], sync_prev=(hl == 1))
                        op("pe", lambda e, hl=hl, c=c, px=px: e.matmul(px[0:64, hl * 64:(hl + 1) * 64], AM[("Aak", hl)][:, c, :], vtok[:, c, hl * 64:(hl + 1) * 64], start=False, stop=True), R=[f"Aak{hl}", "r_vtok"], W=[pkx], sync_prev=(hl == 1))
                    op("act", lambda e, px=px: e.activation(out=xsb[:], in_=px[0:64, 0:128], func=AF.Copy), R=[pkx], W=["r_xsb"])
                    pu, pku = ps()
                    for hl in range(2):
                        op("pe", lambda e, hl=hl, c=c, pu=pu: e.matmul(pu[0:64, hl * 64:(hl + 1) * 64], AM[("Minv", hl)][:, c, :], xsb[:, hl * 64:(hl + 1) * 64], start=True, stop=True), R=[f"Minv{hl}", "r_xsb"], W=[pku])
                    op("act", lambda e, pu=pu: e.activation(out=usb[:], in_=pu[0:64, 0:128], func=AF.Copy), R=[pku], W=["r_usb"])
                    if full:
                        py, pky = ps()
                        for hl in range(2):
                            pb = slice(hl * 64, (hl + 1) * 64)
                            o_ = py[0:64, hl * 64:(hl + 1) * 64]
                            op("pe", lambda e, o_=o_, pb=pb, c=c: e.matmul(o_, ar[pb, c, 64:128], Sb[pb, hp, :], start=True, stop=False), R=["r_ar", "Sb"], W=[pky], sync_prev=(hl == 1))
                            op("pe", lambda e, o_=o_, hl=hl, c=c: e.matmul(o_, AM[("Abr", hl)][:, c, :], usb[:, hl * 64:(hl + 1) * 64], start=False, stop=False), R=[f"Abr{hl}", "r_usb"], W=[pky], sync_prev=(hl == 1))
                            op("pe", lambda e, o_=o_, hl=hl, c=c: e.matmul(o_, AM[("Akr", hl)][:, c, :], vtok[:, c, hl * 64:(hl + 1) * 64], start=False, stop=True), R=[f"Akr{hl}", "r_vtok"], W=[pky])
                        op("act", lambda e, py=py, c=c: e.activation(out=ytok[:, c, :], in_=py[0:64, 0:128], func=AF.Copy), R=[pky], W=["r_ytok"])
                    pS, pkS = ps()
                    op("pe", lambda e, c=c, pS=pS: e.matmul(pS[:, 0:128], bttok[:, c, :], usb[:], start=True, stop=False), R=["r_bttok", "r_usb"], W=[pkS])
                    op("pe", lambda e, c=c, pS=pS: e.matmul(pS[:, 0:128], kttok[:, c, :], vtok[:, c, :], start=False, stop=True), R=["r_kttok", "r_vtok"], W=[pkS])
                    wc = c * 64 + 63
                    op("act", lambda e, wc=wc: e.activation(out=S1[:], in_=Sf[:, hp, :], func=AF.Identity, scale=Fm["ep"][:, wc:wc + 1]), R=["Sf", fk("ep")], W=["r_S1"])
                    for hl in range(2):
                        pb = slice(hl * 64, (hl + 1) * 64)
                        op("dve", lambda e, pb=pb, hl=hl, wc=wc, pS=pS: e.scalar_tensor_tensor(out=Sf[pb, hp, :], in0=pS[pb, hl * 64:(hl + 1) * 64], scalar=Fm["ep"][pb, wc:wc + 1], in1=S1[pb, :], op0=ALU.mult, op1=ALU.add), R=[pkS, fk("ep"), "r_S1"], W=["Sf"])
                    op("act", lambda e: e.activation(out=Sb[:, hp, :], in_=Sf[:, hp, :], func=AF.Copy), R=["Sf"], W=["Sb"])
                if full and LV >= 6:
                    y16 = ytok[:].rearrange("p c (h v) -> p (c h) v", v=64)
                    op("dve", lambda e: e.tensor_reduce(out=sm["s1"][:], in_=y16, axis=AX.X, op=ALU.add), R=["r_ytok"], W=["r_ss1"])
                    op("act", lambda e: e.activation(out=ysq[:], in_=ytok[:], func=AF.Square), R=["r_ytok"], W=["r_ysq"])
                    op("dve", lambda e: e.tensor_reduce(out=sm["s2"][:], in_=ysq[:].rearrange("p c (h v) -> p (c h) v", v=64), axis=AX.X, op=ALU.add), R=["r_ysq"], W=["r_ss2"])
                    op("dve", lambda e: e.tensor_scalar(out=sm["mn"][:], in0=sm["s1"][:], scalar1=1.0 / 64, scalar2=None, op0=ALU.mult), R=["r_ss1"], W=["r_smn"])
                    op("dve", lambda e: e.tensor_tensor(out=sm["vr"][:], in0=sm["mn"][:], in1=sm["mn"][:], op=ALU.mult), R=["r_smn"], W=["r_svr"])
                    op("dve", lambda e: e.scalar_tensor_tensor(out=sm["vr"][:], in0=sm["s2"][:], scalar=1.0 / 64, in1=sm["vr"][:], op0=ALU.mult, op1=ALU.subtract), R=["r_ss2", "r_svr"], W=["r_svr"])
                    op("act", lambda e: e.activation(out=sm["rs"][:], in_=sm["vr"][:], func=AF.Sqrt, bias=64e-5), R=["r_svr"], W=["r_srs"])
                    op("dve", lambda e: e.reciprocal(out=sm["rs"][:], in_=sm["rs"][:]), R=["r_srs"], W=["r_srs"])
                    op("dve", lambda e: e.scalar_tensor_tensor(out=sm["nm"][:], in0=sm["mn"][:], scalar=-1.0, in1=sm["rs"][:], op0=ALU.mult, op1=ALU.mult), R=["r_smn", "r_srs"], W=["r_snm"])
                    pT, pkT = ps()
                    for c in range(8):
                        for hl in range(2):
                            j = c * 2 + hl
                            op("act", lambda e, c=c, hl=hl, j=j: e.activation(out=yn[:, hl * 64:(hl + 1) * 64], in_=ytok[:, c, hl * 64:(hl + 1) * 64], func=AF.Identity, scale=sm["rs"][:, j:j + 1], bias=sm["nm"][:, j:j + 1]), R=["r_ytok", "r_srs", "r_snm"], W=["r_yn"])
                        op("dve", lambda e: e.tensor_tensor(out=yn[:], in0=yn[:], in1=lnw[:], op=ALU.mult), R=["r_yn", "lnw"], W=["r_yn"])
                        op("dve", lambda e: e.tensor_tensor(out=yn[:], in0=yn[:], in1=lnb[:], op=ALU.add), R=["r_yn", "lnb"], W=["r_yn"])
                        for hl in range(2):
                            j = c * 2 + hl
                            op("dve", lambda e, c=c, hl=hl, j=j: e.scalar_tensor_tensor(out=yn[:, hl * 64:(hl + 1) * 64], in0=vtok[:, c, hl * 64:(hl + 1) * 64], scalar=bon[:, j:j + 1], in1=yn[:, hl * 64:(hl + 1) * 64], op0=ALU.mult, op1=ALU.add), R=["r_vtok", "r_bon", "r_yn"], W=["r_yn"])
                        op("dve", lambda e, c=c: e.tensor_tensor(out=otok[:, c, :], in0=yn[:], in1=gtok[:, c, :], op=ALU.mult), R=["r_yn", "r_gtok"], W=["r_otok"])
                        op("pe", lambda e, c=c, pT=pT: e.matmul(pT[:, c * 64:(c + 1) * 64], otok[:, c, :], ident[0:64, 0:64], start=True, stop=True), R=["r_otok", "identb"], W=[pkT])
                    op("act", lambda e, pT=pT, ts_=ts_: e.activation(out=mixed[:, 8 + hp, ts_], in_=pT[:], func=AF.Copy), R=[pkT], W=["mixed"])
        phase_end()
        st.close()

    def outproj(half):
        st = contextlib.ExitStack()
        wo = sb(st, "wo", [128, 16, 512], BF16)
        xt = sb(st, "xt", [128, 512]); hq = sb(st, "hq", [128, 512])
        w_out = I["w_out"].rearrange("(c p) n -> p c n", p=128)
        for db in range(4):
            dma("pool", wo[:], w_out[:, :, db * 512:(db + 1) * 512], W=["wo"], stream="g")
            for tt in range(8):
                r0 = half * TP + tt * 128
                ph, pkh = ps()
                for cc in range(16):
                    op("pe", lambda e, cc=cc, tt=tt, ph=ph: e.matmul(ph[:], mixed[:, cc, tt * 128:(tt + 1) * 128], wo[:, cc, :], start=(cc == 0), stop=(cc == 15)), R=["mixed", "wo"], W=[pkh])
                dma("sp", xt[:], I["xtok"][r0:r0 + 128, db * 512:(db + 1) * 512], W=["xt"], stream="x")
                op("dve", lambda e, ph=ph: e.tensor_tensor(out=hq[:], in0=ph[:], in1=xt[:], op=ALU.add), R=[pkh, "xt"], W=["hq"])
                dma("sp", h_scr[r0:r0 + 128, db * 512:(db + 1) * 512], hq[:], R=["hq"], W=["h_scr"], stream="h")
        hrow = sb(st, "hrow", [128, D]); hn = sb(st, "hn", [128, D], BF16); junk = sb(st, "junk", [128, D], BF16)
        hT = sb(st, "hT", [128, 16, 128]); gbc = sb(st, "gffn_bc", [128, D])
        wrg = sb(st, "wrg", [128, 16, 72]); gpc = sb(st, "gffn_pc", [128, 16]); brb = sb(st, "brb", [128, 72])
        ltri = sb(st, "ltri", [128, 128]); onesf = sb(st, "onesf2", [128, 128]); gbase = sb(st, "gbase", [128, 8])
        R_ = {n: sb(st, "rt_" + n, [128, 8]) for n in ("oh", "eg", "sel", "mk1", "sel2", "mk2", "g8", "pos")}
        lg = sb(st, "rt_lg", [128, 72])
        V = {n: sb(st, "rv_" + n, [128, 1]) for n in ("ssq", "rstd", "gmax", "ngmax", "sume", "gp", "m1", "m2", "dm", "w1", "w2", "dest")}
        dma("sp", gbc[:], I["gffn_bc"], W=["gbc"], stream="c")
        dma("sp", gpc[:], I["gffn_pc"], W=["gpc"], stream="c")
        dma("sp", brb[:], I["br_bc"], W=["brb"], stream="c")
        dma("sp", ltri[:], I["ltri"], W=["ltri"], stream="c")
        dma("sp", gbase[:], I["gbase"], W=["gbase"], stream="c")
        dma("sp", wrg[:], I["wr"].rearrange("(c p) n -> p c n", p=128), W=["wrg"], stream="c")
        op("dve", lambda e: e.memset(onesf[:], 1.0), W=["onesf2"])
        for c in range(16):
            op("dve", lambda e, c=c: e.tensor_scalar(out=wrg[:, c, :], in0=wrg[:, c, :], scalar1=gpc[:, c:c + 1], scalar2=None, op0=ALU.mult), R=["wrg", "gpc"], W=["wrg"])
        v = lambda n: V[n]
        for tt in range(8):
            r0 = half * TP + tt * 128
            col = half * 8 + tt
            dma("sp", hrow[:], h_scr[r0:r0 + 128, :], R=["h_scr"], W=["hrow"], stream="h")
            op("act", lambda e: e.activation(out=junk[:], in_=hrow[:], func=AF.Square, accum_out=v("ssq")[:]), R=["hrow"], W=["junk", "rv_ssq"])
            op("act", lambda e: e.activation(out=v("rstd")[:], in_=v("ssq")[:], func=AF.Sqrt, scale=1.0 / D, bias=1e-6), R=["rv_ssq"], W=["rv_rstd"])
            op("dve", lambda e: e.reciprocal(out=v("rstd")[:], in_=v("rstd")[:]), R=["rv_rstd"], W=["rv_rstd"])
            op("dve", lambda e: e.scalar_tensor_tensor(out=hn[:], in0=hrow[:], scalar=v("rstd")[:], in1=gbc[:], op0=ALU.mult, op1=ALU.mult), R=["hrow", "rv_rstd", "gbc"], W=["hn"])
            for q4 in range(4):
                pt, pk = ps()
                for j in range(4):
                    dc = q4 * 4 + j
                    op("pe", lambda e, dc=dc, j=j, pt=pt: e.matmul(pt[:, j * 128:(j + 1) * 128], hrow[:, dc * 128:(dc + 1) * 128], identf[:], start=True, stop=True), R=["hrow", "identf"], W=[pk])
                op("act", lambda e, q4=q4, pt=pt: e.activation(out=hT[:, q4 * 4:(q4 + 1) * 4, :], in_=pt[:].rearrange("p (c t) -> p c t", t=128), func=AF.Copy), R=[pk], W=["hT"])
            pl, pkl = ps()
            for dc in range(16):
                op("pe", lambda e, dc=dc, pl=pl: e.matmul(pl[:, 0:72], hT[:, dc, :], wrg[:, dc, :], start=(dc == 0), stop=(dc == 15)), R=["hT", "wrg"], W=[pkl])
            op("dve", lambda e, pl=pl: e.scalar_tensor_tensor(out=lg[:], in0=pl[:, 0:72], scalar=v("rstd")[:], in1=brb[:], op0=ALU.mult, op1=ALU.add), R=[pkl, "rv_rstd", "brb"], W=["rt_lg"])
            op("dve", lambda e: e.tensor_reduce(out=v("gmax")[:], in_=lg[:, 0:8], axis=AX.X, op=ALU.max), R=["rt_lg"], W=["rv_gmax"])
            op("dve", lambda e: e.tensor_scalar(out=R_["oh"][:], in0=lg[:, 0:8], scalar1=v("gmax")[:], scalar2=None, op0=ALU.is_equal), R=["rt_lg", "rv_gmax"], W=["rt_oh"])
            op("dve", lambda e: e.tensor_scalar(out=v("ngmax")[:], in0=v("gmax")[:], scalar1=-1.0, scalar2=None, op0=ALU.mult), R=["rv_gmax"], W=["rv_ngmax"])
            op("act", lambda e: e.activation(out=R_["eg"][:], in_=lg[:, 0:8], func=AF.Exp, bias=v("ngmax")[:], accum_out=v("sume")[:]), R=["rt_lg", "rv_ngmax"], W=["rt_eg", "rv_sume"])
            op("dve", lambda e: e.reciprocal(out=v("gp")[:], in_=v("sume")[:]), R=["rv_sume"], W=["rv_gp"])
            for g in range(8):
                if g == 0:
                    op("dve", lambda e: e.tensor_scalar(out=R_["sel"][:], in0=lg[:, 8:16], scalar1=R_["oh"][:, 0:1], scalar2=None, op0=ALU.mult), R=["rt_lg", "rt_oh"], W=["rt_sel"])
                else:
                    op("dve", lambda e, g=g: e.scalar_tensor_tensor(out=R_["sel"][:], in0=lg[:, 8 + g * 8:16 + g * 8], scalar=R_["oh"][:, g:g + 1], in1=R_["sel"][:], op0=ALU.mult, op1=ALU.add), R=["rt_lg", "rt_oh", "rt_sel"], W=["rt_sel"])
            op("dve", lambda e: e.tensor_reduce(out=v("m1")[:], in_=R_["sel"][:], axis=AX.X, op=ALU.max), R=["rt_sel"], W=["rv_m1"])
            op("dve", lambda e: e.tensor_scalar(out=R_["mk1"][:], in0=R_["sel"][:], scalar1=v("m1")[:], scalar2=None, op0=ALU.is_equal), R=["rt_sel", "rv_m1"], W=["rt_mk1"])
            op("dve", lambda e: e.scalar_tensor_tensor(out=R_["sel2"][:], in0=R_["mk1"][:], scalar=-1e30, in1=R_["sel"][:], op0=ALU.mult, op1=ALU.add), R=["rt_mk1", "rt_sel"], W=["rt_sel2"])
            op("dve", lambda e: e.tensor_reduce(out=v("m2")[:], in_=R_["sel2"][:], axis=AX.X, op=ALU.max), R=["rt_sel2"], W=["rv_m2"])
            op("dve", lambda e: e.tensor_scalar(out=R_["mk2"][:], in0=R_["sel2"][:], scalar1=v("m2")[:], scalar2=None, op0=ALU.is_equal), R=["rt_sel2", "rv_m2"], W=["rt_mk2"])
            op("dve", lambda e: e.tensor_tensor(out=v("dm")[:], in0=v("m2")[:], in1=v("m1")[:], op=ALU.subtract), R=["rv_m1", "rv_m2"], W=["rv_dm"])
            op("act", lambda e: e.activation(out=v("dm")[:], in_=v("dm")[:], func=AF.Exp), R=["rv_dm"], W=["rv_dm"])
            op("dve", lambda e: e.tensor_scalar(out=v("w1")[:], in0=v("dm")[:], scalar1=1.0, scalar2=None, op0=ALU.add), R=["rv_dm"], W=["rv_w1"])
            op("dve", lambda e: e.reciprocal(out=v("w1")[:], in_=v("w1")[:]), R=["rv_w1"], W=["rv_w1"])
            op("dve", lambda e: e.tensor_tensor(out=v("w1")[:], in0=v("w1")[:], in1=v("gp")[:], op=ALU.mult), R=["rv_w1", "rv_gp"], W=["rv_w1"])
            op("dve", lambda e: e.tensor_tensor(out=v("w2")[:], in0=v("w1")[:], in1=v("dm")[:], op=ALU.mult), R=["rv_w1", "rv_dm"], W=["rv_w2"])
            op("dve", lambda e: e.tensor_scalar(out=R_["g8"][:], in0=R_["mk1"][:], scalar1=v("w1")[:], scalar2=None, op0=ALU.mult), R=["rt_mk1", "rv_w1"], W=["rt_g8"])
            op("dve", lambda e: e.scalar_tensor_tensor(out=R_["g8"][:], in0=R_["mk2"][:], scalar=v("w2")[:], in1=R_["g8"][:], op0=ALU.mult, op1=ALU.add), R=["rt_mk2", "rv_w2", "rt_g8"], W=["rt_g8"])
            pp, pkp = ps()
            op("pe", lambda e, pp=pp: e.matmul(pp[:, 0:8], ltri[:], R_["oh"][:], start=True, stop=False), R=["ltri", "rt_oh"], W=[pkp])
            op("pe", lambda e, pp=pp: e.matmul(pp[:, 0:8], onesf[:], ohacc[:], start=False, stop=True), R=["onesf2", "ohacc"], W=[pkp])
            op("dve", lambda e, pp=pp: e.tensor_tensor(out=R_["pos"][:], in0=pp[:, 0:8], in1=gbase[:], op=ALU.add), R=[pkp, "gbase"], W=["rt_pos"])
            op("dve", lambda e: e.tensor_tensor(out=R_["pos"][:], in0=R_["pos"][:], in1=R_["oh"][:], op=ALU.mult), R=["rt_pos", "rt_oh"], W=["rt_pos"])
            op("dve", lambda e: e.tensor_reduce(out=v("dest")[:], in_=R_["pos"][:], axis=AX.X, op=ALU.add), R=["rt_pos"], W=["rv_dest"])
            op("dve", lambda e, col=col: e.tensor_copy(out=dest_i[:, col:col + 1], in_=v("dest")[:]), R=["rv_dest"], W=["dest_i"])
            op("dve", lambda e: e.tensor_tensor(out=ohacc[:], in0=ohacc[:], in1=R_["oh"][:], op=ALU.add), R=["ohacc", "rt_oh", pkp], W=["ohacc"])
            s.dmafn("pool", lambda e, col=col: e.indirect_dma_start(out=hn_scr, out_offset=bass.IndirectOffsetOnAxis(ap=dest_i[:, col:col + 1], axis=0), in_=hn[:], in_offset=None), R=["hn", "dest_i"], W=["hn_scr"], stream="s")
            s.dmafn("pool", lambda e, col=col: e.indirect_dma_start(out=gt_scr, out_offset=bass.IndirectOffsetOnAxis(ap=dest_i[:, col:col + 1], axis=0), in_=R_["g8"][:], in_offset=None), R=["rt_g8", "dest_i"], W=["gt_scr"], stream="s")
        phase_end()
        st.close()

    def moe_phase():
        st = contextlib.ExitStack()
        hntok = sb(st, "hntok", [128, 3, D], BF16); hnT = sb(st, "hnT", [128, 16, CAP], BF16)
        gts = sb(st, "gts", [128, 3, 8]); yacc = sb(st, "yacc", [128, 3, D])
        act_ = sb(st, "actt", [128, 4, CAP], BF16); sl = sb(st, "silu", [128, CAP])
        WG = [sb(st, f"wg{i}", [128, 16, 512], BF16) for i in range(2)]
        WU = [sb(st, f"wu{i}", [128, 16, 512], BF16) for i in range(2)]
        WD = [sb(st, f"wd{i}", [128, 4, D], BF16) for i in range(2)]
        for g in range(8):
            for st3 in range(3):
                r0 = g * CAP + st3 * 128
                dma("sp", hntok[:, st3, :], hn_scr[r0:r0 + 128, :], R=["hn_scr"], W=["hntok"], stream="m")
                dma("sp", gts[:, st3, :], gt_scr[r0:r0 + 128, :], R=["gt_scr"], W=["gts"], stream="m")
            for dc in range(16):
                pt, pk = ps()
                for st3 in range(3):
                    op("pe", lambda e, st3=st3, dc=dc, pt=pt: e.matmul(pt[:, st3 * 128:(st3 + 1) * 128], hntok[:, st3, dc * 128:(dc + 1) * 128], ident[:], start=True, stop=True), R=["hntok", "identb"], W=[pk])
                op("act", lambda e, dc=dc, pt=pt: e.activation(out=hnT[:, dc, :], in_=pt[:, 0:CAP], func=AF.Copy), R=[pk], W=["hnT"])
            for e8 in range(8):
                E = g * 8 + e8
                b = E % 2
                dma("pool", WG[b][:], I["w_gate"][E].rearrange("(c p) n -> p c n", p=128), W=[f"wg{b}"], stream="e")
                dma("pool", WU[b][:], I["w_up"][E].rearrange("(c p) n -> p c n", p=128), W=[f"wu{b}"], stream="e")
                dma("pool", WD[b][:], I["w_down"][E].rearrange("(c p) n -> p c n", p=128), W=[f"wd{b}"], stream="e")
                for hc in range(4):
                    pg, pkg = ps()
                    pu, pku = ps()
                    for dc in range(16):
                        op("pe", lambda e, dc=dc, hc=hc, pg=pg, b=b: e.matmul(pg[:, 0:CAP], WG[b][:, dc, hc * 128:(hc + 1) * 128], hnT[:, dc, :], start=(dc == 0), stop=(dc == 15)), R=[f"wg{b}", "hnT"], W=[pkg])
                    for dc in range(16):
                        op("pe", lambda e, dc=dc, hc=hc, pu=pu, b=b: e.matmul(pu[:, 0:CAP], WU[b][:, dc, hc * 128:(hc + 1) * 128], hnT[:, dc, :], start=(dc == 0), stop=(dc == 15)), R=[f"wu{b}", "hnT"], W=[pku])
                    op("act", lambda e, pg=pg: e.activation(out=sl[:], in_=pg[:, 0:CAP], func=AF.Silu), R=[pkg], W=["silu"])
                    op("dve", lambda e, hc=hc, pu=pu: e.tensor_tensor(out=act_[:, hc, :], in0=sl[:], in1=pu[:, 0:CAP], op=ALU.mult), R=["silu", pku], W=["actt"])
                for st3 in range(3):
                    for db in range(4):
                        pd, pkd = ps()
                        for hc in range(4):
                            op("pe", lambda e, hc=hc, st3=st3, db=db, pd=pd, b=b: e.matmul(pd[:], act_[:, hc, st3 * 128:(st3 + 1) * 128], WD[b][:, hc, db * 512:(db + 1) * 512], start=(hc == 0), stop=(hc == 3)), R=["actt", f"wd{b}"], W=[pkd])
                        if e8 == 0:
                            op("dve", lambda e, st3=st3, db=db, pd=pd, e8=e8: e.tensor_scalar(out=yacc[:, st3, db * 512:(db + 1) * 512], in0=pd[:], scalar1=gts[:, st3, e8:e8 + 1], scalar2=None, op0=ALU.mult), R=[pkd, "gts"], W=["yacc"])
                        else:
                            op("dve", lambda e, st3=st3, db=db, pd=pd, e8=e8: e.scalar_tensor_tensor(out=yacc[:, st3, db * 512:(db + 1) * 512], in0=pd[:], scalar=gts[:, st3, e8:e8 + 1], in1=yacc[:, st3, db * 512:(db + 1) * 512], op0=ALU.mult, op1=ALU.add), R=[pkd, "gts", "yacc"], W=["yacc"])
            for st3 in range(3):
                r0 = g * CAP + st3 * 128
                dma("sp", y_scr[r0:r0 + 128, :], yacc[:, st3, :], R=["yacc"], W=["y_scr"], stream="m")
        phase_end()
        st.close()
        st = contextlib.ExitStack()
        ym = sb(st, "ym", [128, D]); hr = sb(st, "hr2", [128, D]); junk = sb(st, "junk2", [128, D], BF16)
        gf = sb(st, "gfin", [128, D]); ssq = sb(st, "fssq", [128, 1]); rs = sb(st, "frs", [128, 1])
        dma("sp", gf[:], I["gfin_bc"], W=["gfin"], stream="c")
        for tt in range(16):
            r0 = tt * 128
            s.dmafn("pool", lambda e, tt=tt: e.indirect_dma_start(out=ym[:], out_offset=None, in_=y_scr, in_offset=bass.IndirectOffsetOnAxis(ap=dest_i[:, tt:tt + 1], axis=0)), R=["y_scr", "dest_i"], W=["ym"], stream="s")
            dma("sp", hr[:], h_scr[r0:r0 + 128, :], R=["h_scr"], W=["hr2"], stream="h")
            op("dve", lambda e: e.tensor_tensor(out=hr[:], in0=hr[:], in1=ym[:], op=ALU.add), R=["hr2", "ym"], W=["hr2"])
            op("act", lambda e: e.activation(out=junk[:], in_=hr[:], func=AF.Square, accum_out=ssq[:]), R=["hr2"], W=["junk2", "fssq"])
            op("act", lambda e: e.activation(out=rs[:], in_=ssq[:], func=AF.Sqrt, scale=1.0 / D, bias=1e-6), R=["fssq"], W=["frs"])
            op("dve", lambda e: e.reciprocal(out=rs[:], in_=rs[:]), R=["frs"], W=["frs"])
            op("dve", lambda e: e.scalar_tensor_tensor(out=ym[:], in0=hr[:], scalar=rs[:], in1=gf[:], op0=ALU.mult, op1=ALU.mult), R=["hr2", "frs", "gfin"], W=["ym"])
            dma("sp", out[r0:r0 + 128, :], ym[:], R=["ym"], W=["out"], stream="o")
        phase_end()
        st.close()

    for p in range(NPASS):
        kA, s5_pass = mixer_pass(p)
        if "s5" in stages:
            s5_pass()
        if "rwkv" in stages:
            rwkv_pass(p, kA)
        if dbg and "mixed" in DBG and p == 2:
            if "rwkv" not in stages:
                op("dve", lambda e: e.memset(mixed[:, 8:16, :], 0.0), W=["mixed"])
            if "s5" not in stages:
                op("dve", lambda e: e.memset(mixed[:, 0:8, :], 0.0), W=["mixed"])
            dma("pool", DBG["mixed"], mixed[:].rearrange("p a b -> p (a b)"), R=["mixed"], stream="dbg")
        phase_end()
        kA["stA"].close()
        if p >= 2 and "moe" in stages:
            outproj(p - 2)
    phase_end()
    stM.close()
    if "moe" in stages:
        moe_phase()
    phase_end()
    es.close()
    return nc


def prep_inputs(inp):
    f = lambda a: np.ascontiguousarray(np.asarray(a, dtype=np.float32))
    x = f(inp["x"])
    pc = lambda v, n: f(np.asarray(v).reshape(n, 128).T)
    bc = lambda v, p: f(np.broadcast_to(np.asarray(v).reshape(1, -1), (p, np.asarray(v).size)))
    lam_re, lam_im = inp["s5_lambda_re"][0], inp["s5_lambda_im"][0]
    st = lambda a: f(np.asarray(a).reshape(32, 128).T)
    ls = np.repeat(np.asarray(inp["s5_log_step"][0]).reshape(32, 2, 1), 64, axis=2)
    b_re, b_im = np.asarray(inp["s5_b_re"][0]), np.asarray(inp["s5_b_im"][0])
    c_re, c_im = np.asarray(inp["s5_c_re"][0]), np.asarray(inp["s5_c_im"][0])

    def expand(a_gph):
        o = np.zeros((128, 32, 128), np.float32)
        for sbk in range(32):
            bl = sbk % 4
            for gg in range(2):
                g = 2 * sbk + gg
                o[gg * 64:(gg + 1) * 64, sbk, (2 * bl + gg) * 16:(2 * bl + gg + 1) * 16] = a_gph[g]
        return o.reshape(128, 32 * 128)

    tri = np.arange(64)[:, None] < np.arange(64)[None, :]
    rep8 = lambda m: f(np.tile(m.astype(np.float32)[:, None, :], (1, 8, 1)).reshape(64, 512))
    bones = np.zeros((128, 128), np.float32); bones[:64, :64] = 1; bones[64:, 64:] = 1
    hsel = np.zeros((128, 2), np.float32); hsel[:64, 0] = 1; hsel[64:, 1] = 1
    rst = np.ones((128, NT), np.float32); rst[:, ::64] = 0
    common = {
        "gmix": pc(inp["norm_mix_g"][0], 16), "w_in": f(inp["w_in"][0]),
        "mu_bc": bc(inp["rwkv_mu"][0], 128), "lam_re": st(lam_re), "lam_im": st(lam_im), "lstep": st(ls),
        "Bx_re": expand(b_re), "Bx_im": expand(b_im),
        "Cx_re": expand(np.transpose(c_re, (0, 2, 1))), "Cx_im": expand(np.transpose(c_im, (0, 2, 1))),
        "s5d": pc(inp["s5_d"][0], 8), "w_glu": f(inp["s5_w_glu"][0]), "bglu": pc(inp["s5_b_glu"][0], 8),
        "tidx": f(np.broadcast_to(np.arange(NT, dtype=np.float32)[None, :], (128, NT))),
        "w0": pc(inp["rwkv_w0"][0], 8), "a0": pc(inp["rwkv_a0"][0], 8), "w2": f(inp["rwkv_w2"][0]),
        "a2": f(inp["rwkv_a2"][0]), "g2": f(inp["rwkv_g2"][0]), "k_k": pc(inp["rwkv_k_k"][0], 8),
        "k_a": pc(inp["rwkv_k_a"][0], 8), "r_k": pc(np.asarray(inp["rwkv_r_k"][0]).reshape(-1), 8),
        "lnw_bc": bc(inp["rwkv_ln_w"][0], 64), "lnb_bc": bc(inp["rwkv_ln_b"][0], 64),
        "w_out": f(inp["w_out"][0]), "gffn_bc": bc(inp["norm_ffn_g"][0], 128), "gffn_pc": pc(inp["norm_ffn_g"][0], 16),
        "wr": f(np.concatenate([np.asarray(inp["w_route_grp"][0]), np.asarray(inp["w_route_exp"][0])], axis=1)),
        "br_bc": bc(np.concatenate([np.asarray(inp["b_route_grp"][0]), np.asarray(inp["b_route_exp"][0])]), 128),
        "w_gate": f(inp["w_gate"][0]), "w_up": f(inp["w_up"][0]), "w_down": f(inp["w_down"][0]),
        "gfin_bc": bc(inp["norm_final_g"], 128),
        "ident": f(np.eye(128)), "maskLT": rep8(tri), "maskLE": rep8(tri | np.eye(64, dtype=bool)), "maskGT": rep8(tri.T),
        "identrep": rep8(np.eye(64)), "rstmask": rst, "bones": bones, "hsel": hsel,
        "ltri": f(np.arange(128)[:, None] < np.arange(128)[None, :]),
        "gbase": f(np.broadcast_to((np.arange(8) * CAP).astype(np.float32)[None, :], (128, 8))),
    }
    maps = []
    for c in range(8):
        b, th = c // 2, c % 2
        xT = np.zeros((D, 4096), np.float32)
        if th == 0:
            xT[:, 2048:] = x[b, :2048].T
        else:
            xT[:, :] = x[b].T
        m = dict(common)
        m["xT"] = xT
        m["xtok"] = f(x[b, th * 2048:(th + 1) * 2048])
        maps.append(m)
    return maps


_NC_CACHE = {}


def kernel(**inputs):
    maps = prep_inputs(inputs)
    if "nc" not in _NC_CACHE:
        _NC_CACHE["nc"] = build_program()
    nc = _NC_CACHE["nc"]
    res = run_bass_kernel_spmd(nc, maps, core_ids=list(range(8)))
    outp = np.zeros((4, 4096, D), np.float32)
    for c in range(8):
        b, th = c // 2, c % 2
        outp[b, th * 2048:(th + 1) * 2048] = np.asarray(res.results[c]["out"], dtype=np.float32)
    return outp
```

```python
import contextlib
import numpy as np
import ml_dtypes
import concourse.bass as bass
import concourse.mybir as mybir
from concourse.bass_utils import run_bass_kernel_spmd

F32 = mybir.dt.float32
BF16 = mybir.dt.bfloat16
I32 = mybir.dt.int32
U32 = mybir.dt.uint32
AF = mybir.ActivationFunctionType
ALU = mybir.AluOpType
AX = mybir.AxisListType

ENGS = ("pe", "act", "dve", "pool", "sp")
DEBUG = {}
EPOCH = 2000
EPOCH_DMA = 250


class Ctr:
    def __init__(self, s, name, inc):
        self.s, self.name, self.inc, self.n, self.sems = s, name, inc, 0, []
        self.E = EPOCH if inc == 1 else EPOCH_DMA

    def tick(self):
        EPOCH = self.E
        ep = self.n // EPOCH
        if ep >= len(self.sems):
            self.sems.append(self.s.es.enter_context(self.s.nc.semaphore(f"{self.name}_{ep}")))
        self.n += 1
        v = (self.n - ep * EPOCH) * self.inc
        return (self.name, ep), self.sems[ep], v

    def cur(self):
        if self.n == 0:
            return None
        EPOCH = self.E
        ep = (self.n - 1) // EPOCH
        return (self.name, ep), self.sems[ep], (self.n - ep * EPOCH) * self.inc


class _Rec:
    def __init__(self):
        self.call = None

    def __getattr__(self, name):
        def f(*a, **k):
            self.call = (name, a, k)
        return f


def _record(fn):
    r = _Rec()
    fn(r)
    assert r.call is not None
    return r.call


class Sched:
    def __init__(self, nc, es):
        self.nc, self.es = nc, es
        self.e = dict(pe=nc.tensor, act=nc.scalar, dve=nc.vector, pool=nc.gpsimd, sp=nc.sync)
        self.ctr = {k: Ctr(self, "e" + k, 1) for k in ENGS}
        self.dctr = {}
        self.prog = {k: [] for k in ENGS}
        self.seen = {k: {} for k in ENGS}
        self.tiles = {}
        self.semh = {}
        self.nins = 0
        self.log = None

    def _wait(self, eng, dep):
        semkey, sem, val = dep
        if semkey[0][0] == "d":
            c = self.dctr[semkey[0][1:]]
            cur = c.cur()
            val = cur[2] if cur[0] == semkey else EPOCH_DMA * 16
        if self.seen[eng].get(semkey, 0) >= val:
            return
        self.seen[eng][semkey] = val
        self.prog[eng].append(("w", sem, val))
        if self.log is not None:
            self.log.append(f"   {eng} WAIT {semkey} >= {val}")

    def _deps(self, eng, R, W):
        deps = []
        for k in R:
            t = self.tiles.get(k)
            if t and t[0]:
                deps.append(t[0])
        for k in W:
            t = self.tiles.get(k)
            if t:
                if t[0]:
                    deps.append(t[0])
                deps.extend(t[1].values())
        for d in deps:
            if eng == "pe" and d[0][0] == "epe":
                continue
            if d[0][0] == "e" + eng and eng in DEBUG.get("noself", ()):
                continue
            self._wait(eng, d)

    def _commit(self, me, R, W):
        for k in R:
            t = self.tiles.setdefault(k, [None, {}])
            t[1][me[0]] = me
        for k in W:
            self.tiles[k] = [me, {}]

    def op(self, eng, fn, R=(), W=(), sync_prev=False):
        self._deps(eng, R, W)
        if sync_prev and self.ctr[eng].cur() is not None:
            self._wait(eng, self.ctr[eng].cur())
        me = self.ctr[eng].tick()
        if self.log is not None:
            self.log.append(f"{eng} #{me[2]} ep{me[0][1]} R={list(R)} W={list(W)}")
        self.prog[eng].append(("i", _record(fn), me[1], 1))
        self._commit(me, R, W)
        self.nins += 1

    def dma(self, q, out, in_, R=(), W=(), stream="d0", **kw):
        stream = stream + "_" + q
        self._deps(q, R, W)
        c = self.dctr.get(stream)
        if c is None:
            c = self.dctr[stream] = Ctr(self, "d" + stream, 16)
        me = c.tick()
        self.prog[q].append(("i", ("dma_start", (), dict(out=out, in_=in_, **kw)), me[1], 16))
        self._commit(me, R, W)
        self.nins += 1

    def dmafn(self, q, fn, R=(), W=(), stream="d0"):
        stream = stream + "_" + q
        self._deps(q, R, W)
        c = self.dctr.get(stream)
        if c is None:
            c = self.dctr[stream] = Ctr(self, "d" + stream, 16)
        me = c.tick()
        self.prog[q].append(("i", _record(fn), me[1], 16))
        self._commit(me, R, W)
        self.nins += 1

    def barrier(self):
        cs = [c.cur() for c in list(self.ctr.values()) + list(self.dctr.values())]
        for eng in ENGS:
            for d in cs:
                if d is not None:
                    self._wait(eng, d)
        self.tiles = {}

    def flush(self):
        prog = self.prog
        self.prog = {k: [] for k in ENGS}
        with self.nc.Block() as block:
            def run(items):
                def f(e):
                    for it in items:
                        if it[0] == "w":
                            e.wait_ge(it[1], it[2])
                        else:
                            name, a, kw = it[1]
                            getattr(e, name)(*a, **kw).then_inc(it[2], it[3])
                return f
            block.tensor(run(prog["pe"]))
            block.scalar(run(prog["act"]))
            block.vector(run(prog["dve"]))
            block.gpsimd(run(prog["pool"]))
            block.sync(run(prog["sp"]))


D = 2048
TP = 1024
NPASS = 4
TF = 2048
NT = 512
PROJ = 4384
CAP = 384
TWO_PI = 6.283185307179586

IN_SPECS = [
    ("xT", (D, 4096)), ("xtok", (TF, D)), ("gmix", (128, 16)), ("w_in", (D, PROJ)),
    ("mu_bc", (128, 3360)), ("lam_re", (128, 32)), ("lam_im", (128, 32)), ("lstep", (128, 32)),
    ("Bx_re", (128, 32 * 128)), ("Bx_im", (128, 32 * 128)), ("Cx_re", (128, 32 * 128)), ("Cx_im", (128, 32 * 128)),
    ("s5d", (128, 8)), ("w_glu", (1024, 1024)), ("bglu", (128, 8)), ("tidx", (128, NT)),
    ("w0", (128, 8)), ("a0", (128, 8)), ("w2", (64, 1024)), ("a2", (64, 1024)), ("g2", (160, 1024)),
    ("k_k", (128, 8)), ("k_a", (128, 8)), ("r_k", (128, 8)), ("lnw_bc", (64, 1024)), ("lnb_bc", (64, 1024)),
    ("w_out", (D, D)), ("gffn_bc", (128, D)), ("wr", (D, 72)), ("br_bc", (128, 72)),
    ("w_gate", (64, D, 512)), ("w_up", (64, D, 512)), ("w_down", (64, 512, D)), ("gfin_bc", (128, D)),
    ("ident", (128, 128)), ("maskLT", (64, 512)), ("maskLE", (64, 512)), ("maskGT", (64, 512)),
    ("identrep", (64, 512)), ("rstmask", (128, NT)), ("bones", (128, 128)), ("hsel", (128, 2)),
    ("ltri", (128, 128)), ("gbase", (128, 8)), ("gffn_pc", (128, 16)), ("mu_pc", (128, 24)),
]


def build_program(dbg=None, stages=("s5", "rwkv", "moe")):
    nc = bass.Bass("TRN2", target_bir_lowering=False)
    es = contextlib.ExitStack()
    s = Sched(nc, es)
    op, dma = s.op, s.dma
    I = {name: nc.dram_tensor(name, list(shape), F32, kind="ExternalInput").ap() for name, shape in IN_SPECS
         if "moe" in stages or name not in ("w_gate", "w_up", "w_down")}
    out = nc.dram_tensor("out", [TF, D], F32, kind="ExternalOutput").ap()
    DBG = {}
    for name, shape in (dbg or {}).items():
        DBG[name] = nc.dram_tensor(name, list(shape), F32, kind="ExternalOutput").ap()
    h_scr = nc.dram_tensor("h_scr", [TF, D], F32, kind="Internal").ap()
    hn_scr = nc.dram_tensor("hn_scr", [8 * CAP, D], BF16, kind="Internal").ap()
    gt_scr = nc.dram_tensor("gt_scr", [8 * CAP, 8], F32, kind="Internal").ap()
    y_scr = nc.dram_tensor("y_scr", [8 * CAP, D], F32, kind="Internal").ap()

    uniq = [0]

    def sb(st, name, shape, dt=F32):
        uniq[0] += 1
        return st.enter_context(nc.sbuf_tensor(f"{name}_u{uniq[0]}", list(shape), dt))

    PS = [es.enter_context(nc.psum_tensor(f"ps{i}", [128, 512], F32)) for i in range(8)]
    psi = [0]
    pinned = set()

    def ps(pin=False):
        while psi[0] in pinned:
            psi[0] = (psi[0] + 1) % 8
        i = psi[0]
        psi[0] = (i + 1) % 8
        if pin:
            pinned.add(i)
        return PS[i], f"ps{i}"

    def unpin(pk):
        pinned.discard(int(pk[2:]))

    def phase_end():
        s.barrier()
        s.flush()

    def tap(name, ap, R):
        if name in DBG:
            dma("pool", DBG[name], ap, R=R, stream="dbg")

    dest_i = sb(es, "dest_i", [128, 16], I32)
    ident = sb(es, "identb", [128, 128], BF16)
    identf = sb(es, "identf", [128, 128], F32)
    dma("pool", ident[:], I["ident"], W=["identb"], stream="c")
    dma("sp", identf[:], I["ident"], W=["identf"], stream="c")

    stZ = contextlib.ExitStack()
    zb = sb(stZ, "zb", [128, D], BF16)
    zf = sb(stZ, "zf", [128, 8])
    op("dve", lambda e: e.memset(zb[:], 0.0), W=["zb"])
    op("dve", lambda e: e.memset(zf[:], 0.0), W=["zf"])
    for r0 in range(0, 8 * CAP, 128):
        dma("sp", hn_scr[r0:r0 + 128, :], zb[:], R=["zb"], W=["hn_scr"], stream="z")
        dma("sp", gt_scr[r0:r0 + 128, :], zf[:], R=["zf"], W=["gt_scr"], stream="z")
    phase_end()
    stZ.close()

    stM = contextlib.ExitStack()
    mixed = sb(stM, "mixed", [128, 16, TP], BF16)
    bc_scr = [nc.dram_tensor(f"bc_scr{i}", [128, 32 * 128], BF16, kind="Internal").ap() for i in range(4)]
    th2pi = sb(stM, "th2pi", [128, 32]); mag = sb(stM, "mag", [128, 32])
    car_re = sb(stM, "car_re", [128, 32]); car_im = sb(stM, "car_im", [128, 32])
    Sf = sb(stM, "Sf", [128, 8, 64]); Sb = sb(stM, "Sb", [128, 8, 64], BF16)
    xprev = sb(stM, "xprev", [128, 16, 1], BF16)
    ohacc = sb(stM, "ohacc", [128, 8])
    prm = {n: sb(stM, "p_" + n, [128, 8]) for n in ("s5d", "bglu", "w0", "a0", "k_k", "k_a", "r_k")}
    gmix = sb(stM, "gmix", [128, 16])
    w2b = sb(stM, "w2b", [64, 1024], BF16); a2b = sb(stM, "a2b", [64, 1024], BF16)
    g2b0 = sb(stM, "g2b0", [128, 1024], BF16); g2b1 = sb(stM, "g2b1", [32, 1024], BF16)
    for n, t in prm.items():
        dma("sp", t[:], I[n], W=["p_" + n], stream="c")
    dma("sp", gmix[:], I["gmix"], W=["gmix"], stream="c")
    dma("pool", w2b[:], I["w2"], W=["w2b"], stream="c")
    dma("pool", a2b[:], I["a2"], W=["a2b"], stream="c")
    dma("pool", g2b0[:], I["g2"][0:128, :], W=["g2b0"], stream="c")
    dma("pool", g2b1[:], I["g2"][128:160, :], W=["g2b1"], stream="c")
    for t_, kk_ in ((car_re, "car_re"), (car_im, "car_im"), (Sf, "Sf"), (Sb, "Sb"), (xprev, "xprev"), (ohacc, "ohacc")):
        op("dve", lambda e, t_=t_: e.memset(t_[:], 0.0), W=[kk_])

    w_in = I["w_in"].rearrange("(c p) n -> p c n", p=128)

    def s5_setup():
        st = contextlib.ExitStack()
        T = {n: sb(st, "s5_" + n, [128, 32]) for n in
             ("lr", "li", "ls", "dt", "lrd", "th", "q", "fr", "sn", "cs", "are", "aim", "nre", "den", "fre", "fim", "t1", "t2")}
        qi = sb(st, "s5_qi", [128, 32], I32)
        BT_re = sb(st, "BT_re", [128, 32, 128], BF16); BT_im = sb(st, "BT_im", [128, 32, 128], BF16)
        CT_re = sb(st, "CT_re", [128, 32, 128], BF16); CT_ni = sb(st, "CT_ni", [128, 32, 128], BF16)
        bre = sb(st, "s5_bre", [128, 1024]); bim = sb(st, "s5_bim", [128, 1024])
        o1 = sb(st, "s5_o1", [128, 128]); o2 = sb(st, "s5_o2", [128, 128], BF16); o3 = sb(st, "s5_o3", [128, 128], BF16)
        dma("sp", T["lr"][:], I["lam_re"], W=["lr"], stream="c")
        dma("sp", T["li"][:], I["lam_im"], W=["li"], stream="c")
        dma("sp", T["ls"][:], I["lstep"], W=["ls"], stream="c")
        A = lambda o, i_, f, **kw: op("act", lambda e: e.activation(out=T[o][:], in_=T[i_][:], func=f, **kw), R=[i_], W=[o])
        TT = lambda o, a, b, o_: op("dve", lambda e: e.tensor_tensor(out=T[o][:], in0=T[a][:], in1=T[b][:], op=o_), R=[a, b], W=[o])
        A("dt", "ls", AF.Exp)
        TT("lrd", "lr", "dt", ALU.mult)
        TT("th", "li", "dt", ALU.mult)
        op("act", lambda e: e.activation(out=mag[:], in_=T["lrd"][:], func=AF.Exp), R=["lrd"], W=["mag"])
        op("dve", lambda e: e.tensor_scalar(out=th2pi[:], in0=T["th"][:], scalar1=1.0 / TWO_PI, scalar2=None, op0=ALU.mult), R=["th"], W=["th2pi"])
        for nm, off in (("sn", 0.0), ("cs", 0.25)):
            op("dve", lambda e, off=off: e.tensor_scalar(out=T["q"][:], in0=th2pi[:], scalar1=off, scalar2=None, op0=ALU.add), R=["th2pi"], W=["q"])
            op("dve", lambda e: e.tensor_copy(out=qi[:], in_=T["q"][:]), R=["q"], W=["qi"])
            op("dve", lambda e: e.tensor_tensor(out=T["fr"][:], in0=T["q"][:], in1=qi[:], op=ALU.subtract), R=["q", "qi"], W=["fr"])
            op("act", lambda e, nm=nm: e.activation(out=T[nm][:], in_=T["fr"][:], func=AF.Sin, scale=TWO_PI), R=["fr"], W=[nm])
        op("dve", lambda e: e.tensor_tensor(out=T["are"][:], in0=mag[:], in1=T["cs"][:], op=ALU.mult), R=["mag", "cs"], W=["are"])
        op("dve", lambda e: e.tensor_tensor(out=T["aim"][:], in0=mag[:], in1=T["sn"][:], op=ALU.mult), R=["mag", "sn"], W=["aim"])
        op("dve", lambda e: e.tensor_scalar(out=T["nre"][:], in0=T["are"][:], scalar1=-1.0, scalar2=None, op0=ALU.add), R=["are"], W=["nre"])
        TT("t1", "lr", "lr", ALU.mult); TT("t2", "li", "li", ALU.mult); TT("den", "t1", "t2", ALU.add)
        op("dve", lambda e: e.reciprocal(out=T["den"][:], in_=T["den"][:]), R=["den"], W=["den"])
        TT("t1", "nre", "lr", ALU.mult); TT("t2", "aim", "li", ALU.mult); TT("fre", "t1", "t2", ALU.add); TT("fre", "fre", "den", ALU.mult)
        TT("t1", "aim", "lr", ALU.mult); TT("t2", "nre", "li", ALU.mult); TT("fim", "t1", "t2", ALU.subtract); TT("fim", "fim", "den", ALU.mult)
        for b8 in range(4):
            dma("sp", bre[:], I["Bx_re"][:, b8 * 1024:(b8 + 1) * 1024], W=["bre"], stream="c")
            dma("sp", bim[:], I["Bx_im"][:, b8 * 1024:(b8 + 1) * 1024], W=["bim"], stream="c")
            for j in range(8):
                b = b8 * 8 + j
                sl = slice(j * 128, (j + 1) * 128)
                op("dve", lambda e, sl=sl, b=b: e.tensor_scalar(out=o1[:], in0=bim[:, sl], scalar1=T["fim"][:, b:b + 1], scalar2=None, op0=ALU.mult), R=["bim", "fim"], W=["o1"])
                op("dve", lambda e, sl=sl, b=b: e.scalar_tensor_tensor(out=o2[:], in0=bre[:, sl], scalar=T["fre"][:, b:b + 1], in1=o1[:], op0=ALU.mult, op1=ALU.subtract), R=["bre", "fre", "o1"], W=["o2"])
                op("dve", lambda e, sl=sl, b=b: e.tensor_scalar(out=o1[:], in0=bre[:, sl], scalar1=T["fim"][:, b:b + 1], scalar2=None, op0=ALU.mult), R=["bre", "fim", "o2"], W=["o1"])
                op("dve", lambda e, sl=sl, b=b: e.scalar_tensor_tensor(out=o3[:], in0=bim[:, sl], scalar=T["fre"][:, b:b + 1], in1=o1[:], op0=ALU.mult, op1=ALU.add), R=["bim", "fre", "o1"], W=["o3"])
                for src, srck, dst, dstk in ((o2, "o2", BT_re, "BT_re"), (o3, "o3", BT_im, "BT_im")):
                    pt, pk = ps()
                    op("pe", lambda e, src=src, pt=pt: e.matmul(pt[:, 0:128], src[:], ident[:], start=True, stop=True), R=[srck, "identb"], W=[pk])
                    op("act", lambda e, dst=dst, pt=pt, b=b: e.activation(out=dst[:, b, :], in_=pt[:, 0:128], func=AF.Copy), R=[pk], W=[dstk])
            dma("sp", bre[:], I["Cx_re"][:, b8 * 1024:(b8 + 1) * 1024], W=["bre"], stream="c")
            dma("sp", bim[:], I["Cx_im"][:, b8 * 1024:(b8 + 1) * 1024], W=["bim"], stream="c")
            op("act", lambda e, b8=b8: e.activation(out=CT_re[:, b8 * 8:(b8 + 1) * 8, :], in_=bre[:].rearrange("p (a b) -> p a b", b=128), func=AF.Copy), R=["bre"], W=["CT_re"])
            op("act", lambda e, b8=b8: e.activation(out=CT_ni[:, b8 * 8:(b8 + 1) * 8, :], in_=bim[:].rearrange("p (a b) -> p a b", b=128), func=AF.Identity, scale=-1.0), R=["bim"], W=["CT_ni"])
        for i_, (t_, k_) in enumerate(((BT_re, "BT_re"), (BT_im, "BT_im"), (CT_re, "CT_re"), (CT_ni, "CT_ni"))):
            dma("sp", bc_scr[i_], t_[:].rearrange("p a b -> p (a b)"), R=[k_], W=["bc_scr"], stream="c")
        phase_end()
        st.close()

    s5_setup()

    def mixer_pass(p):
        full = p >= 2
        stA = contextlib.ExitStack()
        xh = sb(stA, "xh", [128, 16, TP + 1], BF16)
        wst = sb(stA, "wst", [128, 16, 128])
        mut = sb(stA, "mut", [128, 128]); omut = sb(stA, "omut", [128, 128])
        stX = contextlib.ExitStack()
        xst = [sb(stX, f"xst{i}", [128, NT]) for i in range(2)]
        sqt = [sb(stX, f"sq{i}", [128, NT], BF16) for i in range(2)]
        rstd = sb(stX, "rstd", [128, NT])
        onesb = sb(stX, "onesb", [128, 128], BF16)
        wt = {}
        op("dve", lambda e: e.memset(onesb[:], 1.0), W=["onesb"])
        op("act", lambda e: e.activation(out=xh[:, :, 0:1], in_=xprev[:], func=AF.Copy), R=["xprev"], W=["xh"])
        for t in range(TP // NT):
            t0 = p * TP + t * NT
            pss, pk = ps()
            for c in range(16):
                xs, xk = xst[c % 2], f"xst{c % 2}"
                dma("sp", xs[:], I["xT"][c * 128:(c + 1) * 128, t0:t0 + NT], W=[xk], stream="x")
                op("act", lambda e, c=c, xs=xs: e.activation(out=sqt[c % 2][:], in_=xs[:], func=AF.Square), R=[xk], W=[f"sq{c % 2}"])
                op("pe", lambda e, c=c: e.matmul(pss[:], onesb[:], sqt[c % 2][:], start=(c == 0), stop=(c == 15)), R=[f"sq{c % 2}", "onesb"], W=[pk])
            op("act", lambda e: e.activation(out=rstd[:], in_=pss[:], func=AF.Sqrt, scale=1.0 / D, bias=1e-6), R=[pk], W=["rstd"])
            op("dve", lambda e: e.reciprocal(out=rstd[:], in_=rstd[:]), R=["rstd"], W=["rstd"])
            for c in range(16):
                xs, xk = xst[c % 2], f"xst{c % 2}"
                dma("sp", xs[:], I["xT"][c * 128:(c + 1) * 128, t0:t0 + NT], W=[xk], stream="x")
                op("dve", lambda e, c=c, t=t, xs=xs: e.scalar_tensor_tensor(
                    out=xh[:, c, 1 + t * NT:1 + (t + 1) * NT], in0=xs[:], scalar=gmix[:, c:c + 1], in1=rstd[:],
                    op0=ALU.mult, op1=ALU.mult), R=[xk, "rstd", "gmix"], W=["xh"])
        op("act", lambda e: e.activation(out=xprev[:], in_=xh[:, :, TP:TP + 1], func=AF.Copy), R=["xh"], W=["xprev"])
        if p == 2:
            tap("xh", xh[:].rearrange("p a b -> p (a b)"), ["xh"])
        phase_end()
        stX.close()

        def load_w(col0, ncols, shifted, slot):
            w1, w0 = wt[(slot, 1)], wt.get((slot, 0))
            dma("sp", wst[:, :, :ncols], w_in[:, :, col0:col0 + ncols], W=["wst"], stream="w")
            if not shifted:
                op("act", lambda e: e.activation(out=w1[:, :, :ncols], in_=wst[:, :, :ncols], func=AF.Copy), R=["wst"], W=[f"w1_{slot}"])
                return (w1, f"w1_{slot}"), None
            m0 = col0 - 1024
            dma("sp", mut[:, :ncols], I["mu_bc"][:, m0:m0 + ncols], W=["mut"], stream="w")
            op("pool", lambda e: e.tensor_scalar(out=omut[:, :ncols], in0=mut[:, :ncols], scalar1=-1.0, scalar2=1.0, op0=ALU.mult, op1=ALU.add), R=["mut"], W=["omut"])
            for c in range(16):
                op("pool", lambda e, c=c: e.tensor_tensor(out=w1[:, c, :ncols], in0=wst[:, c, :ncols], in1=omut[:, :ncols], op=ALU.mult), R=["wst", "omut"], W=[f"w1_{slot}"])
                op("pool", lambda e, c=c: e.tensor_tensor(out=w0[:, c, :ncols], in0=wst[:, c, :ncols], in1=mut[:, :ncols], op=ALU.mult), R=["wst", "mut"], W=[f"w0_{slot}"])
            return (w1, f"w1_{slot}"), (w0, f"w0_{slot}")

        def proj_fm(pst, pk, W1, W0, ncols, t):
            n = 16 * (2 if W0 else 1)
            i = 0
            for c in range(16):
                op("pe", lambda e, c=c, i=i: e.matmul(pst[0:ncols, :], W1[0][:, c, 0:ncols], xh[:, c, 1 + t * NT:1 + (t + 1) * NT], start=(i == 0), stop=(i == n - 1)), R=["xh", W1[1]], W=[pk])
                i += 1
                if W0:
                    op("pe", lambda e, c=c, i=i: e.matmul(pst[0:ncols, :], W0[0][:, c, 0:ncols], xh[:, c, t * NT:(t + 1) * NT], start=False, stop=(i == n - 1)), R=["xh", W0[1]], W=[pk])
                    i += 1

        def s5_pass():
            st = contextlib.ExitStack()
            F = {n: sb(st, "f_" + n, [128, NT]) for n in ("zr", "zi", "t1", "rr", "ri", "magt", "y1", "x2")}
            ub = sb(st, "f_ub", [128, NT], BF16); sre = sb(st, "f_sre", [128, NT], BF16); sim = sb(st, "f_sim", [128, NT], BF16)
            tidx = sb(st, "tidx", [128, NT]); onesf = sb(st, "onesf", [128, NT])
            BT_re = sb(st, "BT_re", [128, 32, 128], BF16); BT_im = sb(st, "BT_im", [128, 32, 128], BF16)
            CT_re = sb(st, "CT_re", [128, 32, 128], BF16); CT_ni = sb(st, "CT_ni", [128, 32, 128], BF16)
            for i_, (t_, k_) in enumerate(((BT_re, "BT_re"), (BT_im, "BT_im"), (CT_re, "CT_re"), (CT_ni, "CT_ni"))):
                dma("sp", t_[:].rearrange("p a b -> p (a b)"), bc_scr[i_], R=["bc_scr"], W=[k_], stream="c")
            wt.update({(v, j): sb(st, f"wt{v}{j}", [128, 16, 128], BF16) for v in range(1) for j in range(2)})
            qb0 = sb(st, "qb0", [128, 32]); qb1 = sb(st, "qb1", [128, 32])
            dma("sp", tidx[:], I["tidx"], W=["tidx"], stream="c")
            op("dve", lambda e: e.memset(onesf[:], 1.0), W=["onesf"])
            if full:
                yg = sb(st, "yg", [128, 8, TP], BF16)
                wg = sb(st, "wglu", [128, 8, 128], BF16)
            TT = lambda eng, o, a, b, o_: op(eng, lambda e: e.tensor_tensor(out=o[0][:], in0=a[0][:], in1=b[0][:], op=o_), R=[a[1], b[1]], W=[o[1]])
            f = lambda n: (F[n], "f_" + n)
            F2 = {(n, i): sb(st, f"f2_{n}{i}", [128, NT]) for n in ("cn", "sn", "q", "uf") for i in range(2)}
            qi2 = [sb(st, f"f2_qi{i}", [128, NT], I32) for i in range(2)]
            units = [(gb, t, bl) for gb in range(8) for t in range(TP // NT) for bl in range(4)]
            ctx = {}

            def pre(gb, t):
                par = (gb * (TP // NT) + t) % 2
                if t == 0:
                    ctx["W1"] = load_w(gb * 128, 128, False, 0)[0]
                W1 = ctx["W1"]
                toff = float(p * TP + t * NT)
                op("dve", lambda e: e.tensor_scalar(out=qb0[:], in0=th2pi[:], scalar1=toff, scalar2=None, op0=ALU.mult), R=["th2pi"], W=["qb"])
                op("dve", lambda e: e.tensor_scalar(out=qb1[:], in0=th2pi[:], scalar1=toff, scalar2=0.25, op0=ALU.mult, op1=ALU.add), R=["th2pi"], W=["qb"])
                pu, pku = ps()
                proj_fm(pu, pku, W1, None, 128, t)
                op("act", lambda e: e.activation(out=ub[:], in_=pu[:], func=AF.Copy), R=[pku], W=["f_ub"])
                info = dict(par=par)
                if full:
                    op("act", lambda e: e.activation(out=F2[("uf", par)][:], in_=pu[:], func=AF.Copy), R=[pku], W=[f"f2_uf{par}"])
                    info["py"], info["pky"] = ps(pin=True)
                ctx[(gb, t)] = info

            def head(i):
                gb, t, bl = units[i]
                if bl == 0:
                    pre(gb, t)
                b = gb * 4 + bl
                u2 = i % 2
                pr, pkr = ps()
                pi_, pki = ps()
                op("pe", lambda e: e.matmul(pr[:], BT_re[:, b, :], ub[:], start=True, stop=True), R=["BT_re", "f_ub"], W=[pkr])
                op("pe", lambda e: e.matmul(pi_[:], BT_im[:, b, :], ub[:], start=True, stop=True), R=["BT_im", "f_ub"], W=[pki])
                for j, (nm, qbt) in enumerate((("sn", qb0), ("cn", qb1))):
                    q_, qk = F2[("q", j)], f"f2_q{j}"
                    o_, ok_ = F2[(nm, u2)], f"f2_{nm}{u2}"
                    op("act", lambda e: e.activation(out=q_[:], in_=tidx[:], func=AF.Identity, scale=th2pi[:, b:b + 1], bias=qbt[:, b:b + 1]), R=["tidx", "th2pi", "qb"], W=[qk])
                    op("pool", lambda e: e.tensor_copy(out=qi2[j][:], in_=q_[:]), R=[qk], W=[f"f2_qi{j}"])
                    op("pool", lambda e: e.tensor_tensor(out=q_[:], in0=q_[:], in1=qi2[j][:], op=ALU.subtract), R=[qk, f"f2_qi{j}"], W=[qk])
                    op("act", lambda e: e.activation(out=o_[:], in_=q_[:], func=AF.Sin, scale=TWO_PI), R=[qk], W=[ok_])
                ctx[i] = (pr, pkr, pi_, pki)

            def body(i):
                gb, t, bl = units[i]
                b = gb * 4 + bl
                u2 = i % 2
                pr, pkr, pi_, pki = ctx.pop(i)
                info = ctx[(gb, t)]
                cn, sn = (F2[("cn", u2)], f"f2_cn{u2}"), (F2[("sn", u2)], f"f2_sn{u2}")
                P = lambda t_, k_: (t_, k_)
                TT("dve", f("zr"), P(pr, pkr), cn, ALU.mult)
                TT("dve", f("t1"), P(pi_, pki), sn, ALU.mult)
                TT("dve", f("zr"), f("zr"), f("t1"), ALU.add)
                TT("dve", f("zi"), P(pi_, pki), cn, ALU.mult)
                TT("dve", f("t1"), P(pr, pkr), sn, ALU.mult)
                TT("dve", f("zi"), f("zi"), f("t1"), ALU.subtract)
                op("act", lambda e: e.activation(out=F["magt"][:], in_=onesf[:], func=AF.Identity, scale=mag[:, b:b + 1]), R=["onesf", "mag"], W=["f_magt"])
                op("dve", lambda e: e.tensor_tensor_scan(out=F["rr"][:], data0=F["magt"][:], data1=F["zr"][:], initial=car_re[:, b:b + 1], op0=ALU.mult, op1=ALU.add), R=["f_magt", "f_zr", "car_re"], W=["f_rr"])
                op("dve", lambda e: e.tensor_tensor_scan(out=F["ri"][:], data0=F["magt"][:], data1=F["zi"][:], initial=car_im[:, b:b + 1], op0=ALU.mult, op1=ALU.add), R=["f_magt", "f_zi", "car_im"], W=["f_ri"])
                op("dve", lambda e: e.tensor_copy(out=car_re[:, b:b + 1], in_=F["rr"][:, NT - 1:NT]), R=["f_rr"], W=["car_re"])
                op("dve", lambda e: e.tensor_copy(out=car_im[:, b:b + 1], in_=F["ri"][:, NT - 1:NT]), R=["f_ri"], W=["car_im"])
                if full:
                    py, pky = info["py"], info["pky"]
                    TT("dve", f("zr"), cn, f("rr"), ALU.mult)
                    TT("dve", f("t1"), sn, f("ri"), ALU.mult)
                    TT("dve", (sre, "f_sre"), f("zr"), f("t1"), ALU.subtract)
                    TT("dve", f("zi"), cn, f("ri"), ALU.mult)
                    TT("dve", f("t1"), sn, f("rr"), ALU.mult)
                    TT("dve", (sim, "f_sim"), f("zi"), f("t1"), ALU.add)
                    op("pe", lambda e: e.matmul(py[:], CT_re[:, b, :], sre[:], start=(bl == 0), stop=False), R=["CT_re", "f_sre"], W=[pky])
                    op("pe", lambda e: e.matmul(py[:], CT_ni[:, b, :], sim[:], start=False, stop=(bl == 3)), R=["CT_ni", "f_sim"], W=[pky])
                    if bl == 3:
                        par = info["par"]
                        sd = prm["s5d"]
                        op("dve", lambda e: e.scalar_tensor_tensor(out=F["y1"][:], in0=F2[("uf", par)][:], scalar=sd[:, gb:gb + 1], in1=py[:], op0=ALU.mult, op1=ALU.add), R=[f"f2_uf{par}", "p_s5d", pky], W=["f_y1"])
                        unpin(pky)
                        op("act", lambda e: e.activation(out=F["x2"][:], in_=F["y1"][:], func=AF.Square), R=["f_y1"], W=["f_x2"])
                        op("dve", lambda e: e.tensor_scalar(out=F["x2"][:], in0=F["x2"][:], scalar1=0.044715, scalar2=1.0, op0=ALU.mult, op1=ALU.add), R=["f_x2"], W=["f_x2"])
                        TT("dve", f("x2"), f("x2"), f("y1"), ALU.mult)
                        op("act", lambda e: e.activation(out=F["x2"][:], in_=F["x2"][:], func=AF.Sigmoid, scale=1.5957691216057308), R=["f_x2"], W=["f_x2"])
                        op("dve", lambda e: e.tensor_tensor(out=yg[:, gb, t * NT:(t + 1) * NT], in0=F["y1"][:], in1=F["x2"][:], op=ALU.mult), R=["f_y1", "f_x2"], W=["yg"])

            head(0)
            for i in range(len(units)):
                if i + 1 < len(units):
                    head(i + 1)
                body(i)
            if full:
                wglu = I["w_glu"].rearrange("(c p) n -> p c n", p=128)
                for oc in range(8):
                    dma("pool", wg[:], wglu[:, :, oc * 128:(oc + 1) * 128], W=["wglu"], stream="g")
                    for t in range(TP // NT):
                        pg, pkg = ps()
                        for kc in range(8):
                            op("pe", lambda e, kc=kc, t=t, pg=pg: e.matmul(pg[:], wg[:, kc, :], yg[:, kc, t * NT:(t + 1) * NT], start=(kc == 0), stop=(kc == 7)), R=["wglu", "yg"], W=[pkg])
                        op("act", lambda e, oc=oc, pg=pg: e.activation(out=F["x2"][:], in_=pg[:], func=AF.Sigmoid, bias=prm["bglu"][:, oc:oc + 1]), R=[pkg, "p_bglu"], W=["f_x2"])
                        op("dve", lambda e, oc=oc, t=t: e.tensor_tensor(out=mixed[:, oc, t * NT:(t + 1) * NT], in0=yg[:, oc, t * NT:(t + 1) * NT], in1=F["x2"][:], op=ALU.mult), R=["yg", "f_x2"], W=["mixed"])
            phase_end()
            st.close()

        k_ = dict(load_w=load_w, proj_fm=proj_fm, xh=xh, stA=stA, wt=wt)
        return k_, s5_pass

    def rwkv_pass(p, kA):
        full = p >= 2
        load_w, proj_fm, xh, wt = kA["load_w"], kA["proj_fm"], kA["xh"], kA["wt"]
        st = contextlib.ExitStack()
        wt.update({(v, j): sb(st, f"wt{v}{j}", [128, 16, 128], BF16) for (v, j) in ((0, 0), (0, 1), (1, 1), (2, 1))})
        mupc = sb(st, "mupc", [128, 24]); omupc = sb(st, "omupc", [128, 24])
        dma("sp", mupc[:], I["mu_pc"], W=["mupc"], stream="c")
        op("dve", lambda e: e.tensor_scalar(out=omupc[:], in0=mupc[:], scalar1=-1.0, scalar2=1.0, op0=ALU.mult, op1=ALU.add), R=["mupc"], W=["omupc"])
        rS = sb(st, "r_rS", [128, NT]); kS = sb(st, "r_kS", [128, NT]); tmu = sb(st, "r_tmu", [128, NT])

        def proj_shift(dst, dk, W1, t, mcol):
            pz, pkz = ps()
            pl, pkl = ps()
            for c in range(16):
                op("pe", lambda e: e.matmul(pz[:], W1[0][:, c, :], xh[:, c, t * NT:t * NT + NT], start=(c == 0), stop=(c == 15)), R=["xh", W1[1]], W=[pkz])
            for c in range(16):
                op("pe", lambda e: e.matmul(pl[:, 0:1], W1[0][:, c, :], xh[:, c, t * NT + NT:t * NT + NT + 1], start=(c == 0), stop=(c == 15)), R=["xh", W1[1]], W=[pkl])
            op("act", lambda e: e.activation(out=tmu[:], in_=pz[:], func=AF.Identity, scale=mupc[:, mcol:mcol + 1]), R=[pkz, "mupc"], W=["r_tmu"])
            op("dve", lambda e: e.scalar_tensor_tensor(out=dst[:, 0:NT - 1], in0=pz[:, 1:NT], scalar=omupc[:, mcol:mcol + 1], in1=tmu[:, 0:NT - 1], op0=ALU.mult, op1=ALU.add), R=[pkz, "omupc", "r_tmu"], W=[dk])
            op("dve", lambda e: e.scalar_tensor_tensor(out=dst[:, NT - 1:NT], in0=pl[:, 0:1], scalar=omupc[:, mcol:mcol + 1], in1=tmu[:, NT - 1:NT], op0=ALU.mult, op1=ALU.add), R=[pkl, "omupc", "r_tmu"], W=[dk])
        mLT = sb(st, "mLT", [64, 512], BF16); mLE = sb(st, "mLE", [64, 512], BF16); mGT = sb(st, "mGT", [64, 512], BF16)
        idrep = sb(st, "idrep", [64, 512], BF16)
        rstm = sb(st, "rstm", [128, NT]); bones = sb(st, "bones", [128, 128], BF16); hsel = sb(st, "hsel", [128, 2], BF16)
        for t_, n_ in ((mLT, "maskLT"), (mLE, "maskLE"), (mGT, "maskGT"), (idrep, "identrep"), (bones, "bones"), (hsel, "hsel")):
            dma("pool", t_[:], I[n_], W=["c_" + n_], stream="c")
        dma("sp", rstm[:], I["rstmask"], W=["rstm"], stream="c")
        CK = ["c_maskLT", "c_maskLE", "c_maskGT", "c_identrep"]
        txw = sb(st, "txw", [64, TP], BF16); xab = sb(st, "xab", [64, TP], BF16)
        sgx0 = sb(st, "sgx0", [128, TP], BF16); sgx1 = sb(st, "sgx1", [32, TP], BF16)
        lnw = sb(st, "lnw", [64, 128]); lnb = sb(st, "lnb", [64, 128])
        Fm = {n: sb(st, "r_" + n, [128, NT]) for n in ("lw", "cw", "ag", "kk", "rn", "kmod", "ep", "en", "ex")}
        ssq = sb(st, "r_ssq", [128, NT], BF16)
        ar = sb(st, "r_ar", [128, 8, 128], BF16)
        btf = sb(st, "r_bt", [128, NT], BF16); ktf = sb(st, "r_kt", [128, NT], BF16)
        rkr = sb(st, "r_rkr", [128, NT], BF16); vf = sb(st, "r_vf", [128, NT], BF16)
        vtok = sb(st, "r_vtok", [64, 8, 128], BF16); bttok = sb(st, "r_bttok", [64, 8, 128], BF16); kttok = sb(st, "r_kttok", [64, 8, 128], BF16)
        gtok = sb(st, "r_gtok", [64, 8, 128], BF16); bon = sb(st, "r_bon", [64, 16])
        AM = {(n, h): sb(st, f"r_{n}{h}", [64, 8, 64], BF16) for n in ("Aab", "Abr", "Aak", "Akr", "Minv") for h in range(2)}
        CH = {(n, h): sb(st, f"r_c{n}{h}", [64, 8, 64], BF16) for n in ("A0", "A1", "T0", "T1", "P0") for h in range(2)}
        xsb = sb(st, "r_xsb", [64, 128], BF16); usb = sb(st, "r_usb", [64, 128], BF16)
        ytok = sb(st, "r_ytok", [64, 8, 128]); ysq = sb(st, "r_ysq", [64, 8, 128]); otok = sb(st, "r_otok", [64, 8, 128], BF16)
        S1 = sb(st, "r_S1", [128, 64]); yn8 = sb(st, "r_yn", [64, 8, 128])
        sm = {n: sb(st, "r_s" + n, [64, 16]) for n in ("s1", "s2", "mn", "vr", "rs", "nm")}

        for col0, ncols, dst, dk, fn in ((4096, 64, txw, "txw", AF.Tanh), (4160, 64, xab, "xab", AF.Identity),
                                         (4224, 128, sgx0, "sgx0", AF.Sigmoid), (4352, 32, sgx1, "sgx1", AF.Sigmoid)):
            if not full and dk.startswith("sgx"):
                continue
            W1, W0 = load_w(col0, ncols, True, 0)
            for t in range(TP // NT):
                pt, pk = ps()
                proj_fm(pt, pk, W1, W0, ncols, t)
                op("act", lambda e, dst=dst, ncols=ncols, t=t, pt=pt, fn=fn: e.activation(out=dst[0:ncols, t * NT:(t + 1) * NT], in_=pt[0:ncols, :], func=fn), R=[pk], W=[dk])

        fk = lambda n: "r_" + n
        LV = DEBUG.get("lv", 9)
        for hp in range(DEBUG.get("nhp", 8)):
            if LV < 2:
                break
            hc = slice(hp * 128, (hp + 1) * 128)
            Wr = load_w(1024 + hp * 128, 128, False, 0)
            Wk = load_w(2048 + hp * 128, 128, False, 1)
            Wv = load_w(3072 + hp * 128, 128, False, 2)
            if full:
                dma("sp", lnw[:], I["lnw_bc"][:, 1024 * 0 + hp * 128:(hp + 1) * 128], W=["lnw"], stream="c")
                dma("sp", lnb[:], I["lnb_bc"][:, hp * 128:(hp + 1) * 128], W=["lnb"], stream="c")
            for t in range(TP // NT):
                ts_ = slice(t * NT, (t + 1) * NT)
                proj_shift(rS, "r_rS", Wr[0], t, hp)
                proj_shift(kS, "r_kS", Wk[0], t, 8 + hp)
                proj_shift(vf, "r_vf", Wv[0], t, 16 + hp)
                pr, pkr, pkk, pkkk = rS, "r_rS", kS, "r_kS"
                pz, pkz = ps()
                op("pe", lambda e, pz=pz, hc=hc, ts_=ts_: e.matmul(pz[:], w2b[:, hc], txw[:, ts_], start=True, stop=True), R=["w2b", "txw"], W=[pkz])
                op("act", lambda e, pz=pz, hp=hp: e.activation(out=Fm["lw"][:], in_=pz[:], func=AF.Sigmoid, bias=prm["w0"][:, hp:hp + 1]), R=[pkz, "p_w0"], W=[fk("lw")])
                op("dve", lambda e: e.tensor_scalar(out=Fm["lw"][:], in0=Fm["lw"][:], scalar1=-0.6065306597126334, scalar2=None, op0=ALU.mult), R=[fk("lw")], W=[fk("lw")])
                op("dve", lambda e: e.tensor_tensor_scan(out=Fm["cw"][:], data0=rstm[:], data1=Fm["lw"][:], initial=0.0, op0=ALU.mult, op1=ALU.add), R=["rstm", fk("lw")], W=[fk("cw")])
                pa, pka = ps()
                op("pe", lambda e, pa=pa, hc=hc, ts_=ts_: e.matmul(pa[:], a2b[:, hc], xab[:, ts_], start=True, stop=True), R=["a2b", "xab"], W=[pka])
                op("act", lambda e, pa=pa, hp=hp: e.activation(out=Fm["ag"][:], in_=pa[:], func=AF.Sigmoid, bias=prm["a0"][:, hp:hp + 1]), R=[pka, "p_a0"], W=[fk("ag")])
                op("act", lambda e, pkk=pkk, hp=hp: e.activation(out=Fm["kk"][:], in_=pkk[:], func=AF.Identity, scale=prm["k_k"][:, hp:hp + 1]), R=[pkkk, "p_k_k"], W=[fk("kk")])
                op("act", lambda e: e.activation(out=ssq[:], in_=Fm["kk"][:], func=AF.Square), R=[fk("kk")], W=["r_ssq"])
                pss, pks = ps()
                op("pe", lambda e, pss=pss: e.matmul(pss[:], bones[:], ssq[:], start=True, stop=True), R=["c_bones", "r_ssq"], W=[pks])
                op("act", lambda e, pss=pss: e.activation(out=Fm["rn"][:], in_=pss[:], func=AF.Sqrt), R=[pks], W=[fk("rn")])
                op("dve", lambda e: e.tensor_scalar(out=Fm["rn"][:], in0=Fm["rn"][:], scalar1=1e-12, scalar2=None, op0=ALU.max), R=[fk("rn")], W=[fk("rn")])
                op("dve", lambda e: e.reciprocal(out=Fm["rn"][:], in_=Fm["rn"][:]), R=[fk("rn")], W=[fk("rn")])
                op("dve", lambda e: e.tensor_tensor(out=Fm["kk"][:], in0=Fm["kk"][:], in1=Fm["rn"][:], op=ALU.mult), R=[fk("kk"), fk("rn")], W=[fk("kk")])
                op("dve", lambda e, hp=hp: e.tensor_scalar(out=Fm["rn"][:], in0=Fm["ag"][:], scalar1=-1.0, scalar2=prm["k_a"][:, hp:hp + 1], op0=ALU.add, op1=ALU.mult), R=[fk("ag"), "p_k_a", fk("kk")], W=[fk("rn")])
                op("dve", lambda e, pkk=pkk: e.scalar_tensor_tensor(out=Fm["kmod"][:], in0=Fm["rn"][:], scalar=1.0, in1=pkk[:], op0=ALU.add, op1=ALU.mult), R=[fk("rn"), pkkk], W=[fk("kmod")])
                op("act", lambda e: e.activation(out=Fm["ep"][:], in_=Fm["cw"][:], func=AF.Exp), R=[fk("cw")], W=[fk("ep")])
                op("act", lambda e: e.activation(out=Fm["en"][:], in_=Fm["cw"][:], func=AF.Exp, scale=-1.0), R=[fk("cw")], W=[fk("en")])
                op("dve", lambda e: e.tensor_tensor(out=Fm["ex"][:], in0=Fm["cw"][:], in1=Fm["lw"][:], op=ALU.subtract), R=[fk("cw"), fk("lw")], W=[fk("ex")])
                op("act", lambda e: e.activation(out=Fm["ex"][:], in_=Fm["ex"][:], func=AF.Exp), R=[fk("ex")], W=[fk("ex")])
                v3 = lambda a: a[:].rearrange("p (c t) -> p c t", t=64)
                op("dve", lambda e, pr=pr: e.tensor_tensor(out=ar[:, :, 64:128], in0=v3(pr), in1=v3(Fm["ep"]), op=ALU.mult), R=[pkr, fk("ep")], W=["r_ar"])
                op("dve", lambda e: e.scalar_tensor_tensor(out=ar[:, :, 0:64], in0=v3(Fm["kk"]), scalar=-1.0, in1=v3(Fm["ex"]), op0=ALU.mult, op1=ALU.mult), R=[fk("kk"), fk("ex")], W=["r_ar"])
                op("dve", lambda e: e.tensor_tensor(out=Fm["rn"][:], in0=Fm["kk"][:], in1=Fm["ag"][:], op=ALU.mult), R=[fk("kk"), fk("ag"), fk("kmod")], W=[fk("rn")])
                op("dve", lambda e: e.tensor_tensor(out=btf[:], in0=Fm["rn"][:], in1=Fm["en"][:], op=ALU.mult), R=[fk("rn"), fk("en")], W=["r_bt"])
                op("dve", lambda e: e.tensor_tensor(out=ktf[:], in0=Fm["kmod"][:], in1=Fm["en"][:], op=ALU.mult), R=[fk("kmod"), fk("en")], W=["r_kt"])
                if full:
                    op("dve", lambda e, pr=pr, hp=hp: e.scalar_tensor_tensor(out=rkr[:], in0=Fm["kmod"][:], scalar=prm["r_k"][:, hp:hp + 1], in1=pr[:], op0=ALU.mult, op1=ALU.mult), R=[fk("kmod"), "p_r_k", pkr], W=["r_rkr"])
                if LV < 3:
                    continue
                def tr(src, srck, dst, dstk):
                    for half in range(2):
                        pt, pk = ps()
                        for c4 in range(4):
                            c = half * 4 + c4
                            op("pe", lambda e, c=c, c4=c4, pt=pt: e.matmul(pt[0:64, c4 * 128:(c4 + 1) * 128], src[:, c * 64:(c + 1) * 64], ident[:], start=True, stop=True), R=[srck, "identb"], W=[pk])
                        op("act", lambda e, half=half, pt=pt: e.activation(out=dst[:, half * 4:(half + 1) * 4, :], in_=pt[0:64, :].rearrange("p (c t) -> p c t", t=128), func=AF.Copy), R=[pk], W=[dstk])
                tr(vf, "r_vf", vtok, "r_vtok"); tr(btf, "r_bt", bttok, "r_bttok"); tr(ktf, "r_kt", kttok, "r_kttok")
                if full:
                    for half in range(2):
                        pt, pk = ps()
                        for c4 in range(4):
                            c = half * 4 + c4
                            tk = slice(t * NT + c * 64, t * NT + (c + 1) * 64)
                            op("pe", lambda e, c4=c4, pt=pt, tk=tk: e.matmul(pt[0:64, c4 * 128:(c4 + 1) * 128], sgx0[:, tk], g2b0[:, hc], start=True, stop=False), R=["sgx0", "g2b0"], W=[pk])
                            op("pe", lambda e, c4=c4, pt=pt, tk=tk: e.matmul(pt[0:64, c4 * 128:(c4 + 1) * 128], sgx1[:, tk], g2b1[:, hc], start=False, stop=True), R=["sgx1", "g2b1"], W=[pk])
                        op("act", lambda e, half=half, pt=pt: e.activation(out=gtok[:, half * 4:(half + 1) * 4, :], in_=pt[0:64, :].rearrange("p (c t) -> p c t", t=128), func=AF.Copy), R=[pk], W=["r_gtok"])
                    pt, pk = ps()
                    for c in range(8):
                        op("pe", lambda e, c=c, pt=pt: e.matmul(pt[0:64, c * 2:(c + 1) * 2], rkr[:, c * 64:(c + 1) * 64], hsel[:], start=True, stop=True), R=["r_rkr", "c_hsel"], W=[pk])
                    op("act", lambda e, pt=pt: e.activation(out=bon[:], in_=pt[0:64, 0:16], func=AF.Copy), R=[pk], W=["r_bon"])
                if LV < 4:
                    continue
                for hl in range(2):
                    pb = slice(hl * 64, (hl + 1) * 64)
                    for src, n1, n2 in ((btf, "Aab", "Abr"), (ktf, "Aak", "Akr")):
                        for half in range(2):
                            pt, pk = ps()
                            for c4 in range(4):
                                c = half * 4 + c4
                                op("pe", lambda e, c=c, c4=c4, pt=pt, src=src: e.matmul(pt[0:64, c4 * 128:(c4 + 1) * 128], src[pb, c * 64:(c + 1) * 64], ar[pb, c, :], start=True, stop=True), R=["r_bt", "r_kt", "r_ar"], W=[pk])
                            pv4 = pt[0:64, :].rearrange("p (c t) -> p c t", t=128)
                            op("dve", lambda e, half=half, pv4=pv4, n1=n1: e.tensor_tensor(out=AM[(n1, hl)][:, half * 4:(half + 1) * 4, :], in0=pv4[:, :, 0:64], in1=mLT[:].rearrange("p (c t) -> p c t", t=64)[:, 0:4, :], op=ALU.mult), R=[pk] + CK, W=[f"{n1}{hl}"])
                            op("dve", lambda e, half=half, pv4=pv4, n2=n2: e.tensor_tensor(out=AM[(n2, hl)][:, half * 4:(half + 1) * 4, :], in0=pv4[:, :, 64:128], in1=mLE[:].rearrange("p (c t) -> p c t", t=64)[:, 0:4, :], op=ALU.mult), R=[pk] + CK, W=[f"{n2}{hl}"])
                    pt, pk = ps()
                    for c in range(8):
                        op("pe", lambda e, c=c, pt=pt: e.matmul(pt[0:64, c * 64:(c + 1) * 64], ar[pb, c, 0:64], btf[pb, c * 64:(c + 1) * 64], start=True, stop=True), R=["r_ar", "r_bt"], W=[pk])
                    op("dve", lambda e, pt=pt: e.tensor_tensor(out=CH[("T0", hl)][:].rearrange("p c t -> p (c t)"), in0=pt[0:64, :], in1=mGT[:], op=ALU.mult), R=[pk] + CK, W=[f"cT0{hl}"])
                    op("dve", lambda e: e.tensor_tensor(out=CH[("P0", hl)][:].rearrange("p c t -> p (c t)"), in0=AM[("Aab", hl)][:].rearrange("p c t -> p (c t)"), in1=idrep[:], op=ALU.add), R=[f"Aab{hl}"] + CK, W=[f"cP0{hl}"])
                stt_ = {hl: dict(A=(AM[("Aab", hl)], f"Aab{hl}"), T=(CH[("T0", hl)], f"cT0{hl}"), P=(CH[("P0", hl)], f"cP0{hl}")) for hl in range(2)}
                for j in range(1, 6):
                    for hl in range(2):
                        (Acur, Ak), (Tcur, Tk), (Pcur, Pk) = stt_[hl]["A"], stt_[hl]["T"], stt_[hl]["P"]
                        An, Ank = CH[(f"A{j % 2}", hl)], f"cA{j % 2}{hl}"
                        Tn, Tnk = CH[(f"T{j % 2}", hl)], f"cT{j % 2}{hl}"
                        if j < 5:
                            pt, pk = ps()
                            for c in range(8):
                                op("pe", lambda e: e.matmul(pt[0:64, c * 64:(c + 1) * 64], Tcur[:, c, :], Acur[:, c, :], start=True, stop=True), R=[Tk, Ak], W=[pk])
                            op("act", lambda e: e.activation(out=An[:].rearrange("p c t -> p (c t)"), in_=pt[0:64, :], func=AF.Copy), R=[pk], W=[Ank])
                        pt2, pk2 = ps()
                        for c in range(8):
                            op("pe", lambda e: e.matmul(pt2[0:64, c * 64:(c + 1) * 64], Acur[:, c, :], Tcur[:, c, :], start=True, stop=True), R=[Tk, Ak], W=[pk2])
                        op("act", lambda e: e.activation(out=Tn[:].rearrange("p c t -> p (c t)"), in_=pt2[0:64, :], func=AF.Copy), R=[pk2], W=[Tnk])
                        pt3, pk3 = ps()
                        for c in range(8):
                            op("pe", lambda e: e.matmul(pt3[0:64, c * 64:(c + 1) * 64], Tn[:, c, :], Pcur[:, c, :], start=True, stop=True), R=[Tnk, Pk], W=[pk3])
                        Pn, Pnk = (AM[("Minv", hl)], f"Minv{hl}") if j % 2 == 1 else (CH[("P0", hl)], f"cP0{hl}")
                        op("dve", lambda e: e.tensor_tensor(out=Pn[:].rearrange("p c t -> p (c t)"), in0=pt3[0:64, :], in1=Pcur[:].rearrange("p c t -> p (c t)"), op=ALU.add), R=[pk3, Pk], W=[Pnk])
                        stt_[hl] = dict(A=(An, Ank), T=(Tn, Tnk), P=(Pn, Pnk))
                if LV < 5:
                    continue
                for c in range(8):
                    px, pkx = ps()
                    for hl in range(2):
                        pb = slice(hl * 64, (hl + 1) * 64)
                        op("pe", lambda e, hl=hl, pb=pb, c=c, px=px: e.matmul(px[0:64, hl * 64:(hl + 1) * 64], ar[pb, c, 0:64], Sb[pb, hp, :], start=True, stop=False), R=["r_ar", "Sb"], W=[pkx], sync_prev=(hl == 1))
                        op("pe", lambda e, hl=hl, c=c, px=px: e.matmul(px[0:64, hl * 64:(hl + 1) * 64], AM[("Aak", hl)][:, c, :], vtok[:, c, hl * 64:(hl + 1) * 64], start=False, stop=True), R=[f"Aak{hl}", "r_vtok"], W=[pkx], sync_prev=(hl == 1))
                    op("act", lambda e, px=px: e.activation(out=xsb[:], in_=px[0:64, 0:128], func=AF.Copy), R=[pkx], W=["r_xsb"])
                    pu, pku = ps()
                    for hl in range(2):
                        op("pe", lambda e, hl=hl, c=c, pu=pu: e.matmul(pu[0:64, hl * 64:(hl + 1) * 64], AM[("Minv", hl)][:, c, :], xsb[:, hl * 64:(hl + 1) * 64], start=True, stop=True), R=[f"Minv{hl}", "r_xsb"], W=[pku])
                    op("act", lambda e, pu=pu: e.activation(out=usb[:], in_=pu[0:64, 0:128], func=AF.Copy), R=[pku], W=["r_usb"])
                    if full:
                        py, pky = ps()
                        for hl in range(2):
                            pb = slice(hl * 64, (hl + 1) * 64)
                            o_ = py[0:64, hl * 64:(hl + 1) * 64]
                            op("pe", lambda e, o_=o_, pb=pb, c=c: e.matmul(o_, ar[pb, c, 64:128], Sb[pb, hp, :], start=True, stop=False), R=["r_ar", "Sb"], W=[pky], sync_prev=(hl == 1))
                            op("pe", lambda e, o_=o_, hl=hl, c=c: e.matmul(o_, AM[("Abr", hl)][:, c, :], usb[:, hl * 64:(hl + 1) * 64], start=False, stop=False), R=[f"Abr{hl}", "r_usb"], W=[pky], sync_prev=(hl == 1))
                            op("pe", lambda e, o_=o_, hl=hl, c=c: e.matmul(o_, AM[("Akr", hl)][:, c, :], vtok[:, c, hl * 64:(hl + 1) * 64], start=False, stop=True), R=[f"Akr{hl}", "r_vtok"], W=[pky])
                        op("act", lambda e, py=py, c=c: e.activation(out=ytok[:, c, :], in_=py[0:64, 0:128], func=AF.Copy), R=[pky], W=["r_ytok"])
                    pS, pkS = ps()
                    op("pe", lambda e, c=c, pS=pS: e.matmul(pS[:, 0:128], bttok[:, c, :], usb[:], start=True, stop=False), R=["r_bttok", "r_usb"], W=[pkS])
                    op("pe", lambda e, c=c, pS=pS: e.matmul(pS[:, 0:128], kttok[:, c, :], vtok[:, c, :], start=False, stop=True), R=["r_kttok", "r_vtok"], W=[pkS])
                    wc = c * 64 + 63
                    op("act", lambda e, wc=wc: e.activation(out=S1[:], in_=Sf[:, hp, :], func=AF.Identity, scale=Fm["ep"][:, wc:wc + 1]), R=["Sf", fk("ep")], W=["r_S1"])
                    for hl in range(2):
                        pb = slice(hl * 64, (hl + 1) * 64)
                        op("dve", lambda e, pb=pb, hl=hl, wc=wc, pS=pS: e.scalar_tensor_tensor(out=Sf[pb, hp, :], in0=pS[pb, hl * 64:(hl + 1) * 64], scalar=Fm["ep"][pb, wc:wc + 1], in1=S1[pb, :], op0=ALU.mult, op1=ALU.add), R=[pkS, fk("ep"), "r_S1"], W=["Sf"])
                    op("act", lambda e: e.activation(out=Sb[:, hp, :], in_=Sf[:, hp, :], func=AF.Copy), R=["Sf"], W=["Sb"])
                if full and LV >= 6:
                    y16 = ytok[:].rearrange("p c (h v) -> p (c h) v", v=64)
                    op("dve", lambda e: e.tensor_reduce(out=sm["s1"][:], in_=y16, axis=AX.X, op=ALU.add), R=["r_ytok"], W=["r_ss1"])
                    op("act", lambda e: e.activation(out=ysq[:], in_=ytok[:], func=AF.Square), R=["r_ytok"], W=["r_ysq"])
                    op("dve", lambda e: e.tensor_reduce(out=sm["s2"][:], in_=ysq[:].rearrange("p c (h v) -> p (c h) v", v=64), axis=AX.X, op=ALU.add), R=["r_ysq"], W=["r_ss2"])
                    op("dve", lambda e: e.tensor_scalar(out=sm["mn"][:], in0=sm["s1"][:], scalar1=1.0 / 64, scalar2=None, op0=ALU.mult), R=["r_ss1"], W=["r_smn"])
                    op("dve", lambda e: e.tensor_tensor(out=sm["vr"][:], in0=sm["mn"][:], in1=sm["mn"][:], op=ALU.mult), R=["r_smn"], W=["r_svr"])
                    op("dve", lambda e: e.scalar_tensor_tensor(out=sm["vr"][:], in0=sm["s2"][:], scalar=1.0 / 64, in1=sm["vr"][:], op0=ALU.mult, op1=ALU.subtract), R=["r_ss2", "r_svr"], W=["r_svr"])
                    op("act", lambda e: e.activation(out=sm["rs"][:], in_=sm["vr"][:], func=AF.Sqrt, bias=64e-5), R=["r_svr"], W=["r_srs"])
                    op("dve", lambda e: e.reciprocal(out=sm["rs"][:], in_=sm["rs"][:]), R=["r_srs"], W=["r_srs"])
                    op("dve", lambda e: e.scalar_tensor_tensor(out=sm["nm"][:], in0=sm["mn"][:], scalar=-1.0, in1=sm["rs"][:], op0=ALU.mult, op1=ALU.mult), R=["r_smn", "r_srs"], W=["r_snm"])
                    pT, pkT = ps()
                    for c in range(8):
                        yk = f"r_yn{c}"
                        for hl in range(2):
                            j = c * 2 + hl
                            op("act", lambda e: e.activation(out=yn8[:, c, hl * 64:(hl + 1) * 64], in_=ytok[:, c, hl * 64:(hl + 1) * 64], func=AF.Identity, scale=sm["rs"][:, j:j + 1], bias=sm["nm"][:, j:j + 1]), R=["r_ytok", "r_srs", "r_snm"], W=[yk])
                    for c in range(8):
                        yk = f"r_yn{c}"
                        op("dve", lambda e: e.tensor_tensor(out=yn8[:, c, :], in0=yn8[:, c, :], in1=lnw[:], op=ALU.mult), R=[yk, "lnw"], W=[yk])
                        op("dve", lambda e: e.tensor_tensor(out=yn8[:, c, :], in0=yn8[:, c, :], in1=lnb[:], op=ALU.add), R=[yk, "lnb"], W=[yk])
                        for hl in range(2):
                            j = c * 2 + hl
                            op("dve", lambda e: e.scalar_tensor_tensor(out=yn8[:, c, hl * 64:(hl + 1) * 64], in0=vtok[:, c, hl * 64:(hl + 1) * 64], scalar=bon[:, j:j + 1], in1=yn8[:, c, hl * 64:(hl + 1) * 64], op0=ALU.mult, op1=ALU.add), R=["r_vtok", "r_bon", yk], W=[yk])
                        op("dve", lambda e: e.tensor_tensor(out=otok[:, c, :], in0=yn8[:, c, :], in1=gtok[:, c, :], op=ALU.mult), R=[yk, "r_gtok"], W=[f"r_otok{c}"])
                        op("pe", lambda e: e.matmul(pT[:, c * 64:(c + 1) * 64], otok[:, c, :], ident[0:64, 0:64], start=True, stop=True), R=[f"r_otok{c}", "identb"], W=[pkT])
                    op("act", lambda e, pT=pT, ts_=ts_: e.activation(out=mixed[:, 8 + hp, ts_], in_=pT[:], func=AF.Copy), R=[pkT], W=["mixed"])
        phase_end()
        st.close()

    def outproj(half):
        st = contextlib.ExitStack()
        wo = sb(st, "wo", [128, 16, 512], BF16)
        xt = sb(st, "xt", [128, 512]); hq = sb(st, "hq", [128, 512])
        w_out = I["w_out"].rearrange("(c p) n -> p c n", p=128)
        for db in range(4):
            dma("pool", wo[:], w_out[:, :, db * 512:(db + 1) * 512], W=["wo"], stream="g")
            for tt in range(8):
                r0 = half * TP + tt * 128
                ph, pkh = ps()
                for cc in range(16):
                    op("pe", lambda e, cc=cc, tt=tt, ph=ph: e.matmul(ph[:], mixed[:, cc, tt * 128:(tt + 1) * 128], wo[:, cc, :], start=(cc == 0), stop=(cc == 15)), R=["mixed", "wo"], W=[pkh])
                dma("sp", xt[:], I["xtok"][r0:r0 + 128, db * 512:(db + 1) * 512], W=["xt"], stream="x")
                op("dve", lambda e, ph=ph: e.tensor_tensor(out=hq[:], in0=ph[:], in1=xt[:], op=ALU.add), R=[pkh, "xt"], W=["hq"])
                dma("sp", h_scr[r0:r0 + 128, db * 512:(db + 1) * 512], hq[:], R=["hq"], W=["h_scr"], stream="h")
        hrow = sb(st, "hrow", [128, D]); hn = sb(st, "hn", [128, D], BF16); junk = sb(st, "junk", [128, D], BF16)
        hT = sb(st, "hT", [128, 16, 128]); gbc = sb(st, "gffn_bc", [128, D])
        wrg = sb(st, "wrg", [128, 16, 72]); gpc = sb(st, "gffn_pc", [128, 16]); brb = sb(st, "brb", [128, 72])
        ltri = sb(st, "ltri", [128, 128]); onesf = sb(st, "onesf2", [128, 128]); gbase = sb(st, "gbase", [128, 8])
        R_ = {n: sb(st, "rt_" + n, [128, 8]) for n in ("oh", "eg", "sel", "mk1", "sel2", "mk2", "g8", "pos")}
        lg = sb(st, "rt_lg", [128, 72])
        V = {n: sb(st, "rv_" + n, [128, 1]) for n in ("ssq", "rstd", "gmax", "ngmax", "sume", "gp", "m1", "m2", "dm", "w1", "w2", "dest")}
        dma("sp", gbc[:], I["gffn_bc"], W=["gbc"], stream="c")
        dma("sp", gpc[:], I["gffn_pc"], W=["gpc"], stream="c")
        dma("sp", brb[:], I["br_bc"], W=["brb"], stream="c")
        dma("sp", ltri[:], I["ltri"], W=["ltri"], stream="c")
        dma("sp", gbase[:], I["gbase"], W=["gbase"], stream="c")
        dma("sp", wrg[:], I["wr"].rearrange("(c p) n -> p c n", p=128), W=["wrg"], stream="c")
        op("dve", lambda e: e.memset(onesf[:], 1.0), W=["onesf2"])
        for c in range(16):
            op("dve", lambda e, c=c: e.tensor_scalar(out=wrg[:, c, :], in0=wrg[:, c, :], scalar1=gpc[:, c:c + 1], scalar2=None, op0=ALU.mult), R=["wrg", "gpc"], W=["wrg"])
        v = lambda n: V[n]
        for tt in range(8):
            r0 = half * TP + tt * 128
            col = half * 8 + tt
            dma("sp", hrow[:], h_scr[r0:r0 + 128, :], R=["h_scr"], W=["hrow"], stream="h")
            op("act", lambda e: e.activation(out=junk[:], in_=hrow[:], func=AF.Square, accum_out=v("ssq")[:]), R=["hrow"], W=["junk", "rv_ssq"])
            op("act", lambda e: e.activation(out=v("rstd")[:], in_=v("ssq")[:], func=AF.Sqrt, scale=1.0 / D, bias=1e-6), R=["rv_ssq"], W=["rv_rstd"])
            op("dve", lambda e: e.reciprocal(out=v("rstd")[:], in_=v("rstd")[:]), R=["rv_rstd"], W=["rv_rstd"])
            op("dve", lambda e: e.scalar_tensor_tensor(out=hn[:], in0=hrow[:], scalar=v("rstd")[:], in1=gbc[:], op0=ALU.mult, op1=ALU.mult), R=["hrow", "rv_rstd", "gbc"], W=["hn"])
            for q4 in range(4):
                pt, pk = ps()
                for j in range(4):
                    dc = q4 * 4 + j
                    op("pe", lambda e, dc=dc, j=j, pt=pt: e.matmul(pt[:, j * 128:(j + 1) * 128], hrow[:, dc * 128:(dc + 1) * 128], identf[:], start=True, stop=True), R=["hrow", "identf"], W=[pk])
                op("act", lambda e, q4=q4, pt=pt: e.activation(out=hT[:, q4 * 4:(q4 + 1) * 4, :], in_=pt[:].rearrange("p (c t) -> p c t", t=128), func=AF.Copy), R=[pk], W=["hT"])
            pl, pkl = ps()
            for dc in range(16):
                op("pe", lambda e, dc=dc, pl=pl: e.matmul(pl[:, 0:72], hT[:, dc, :], wrg[:, dc, :], start=(dc == 0), stop=(dc == 15)), R=["hT", "wrg"], W=[pkl])
            op("dve", lambda e, pl=pl: e.scalar_tensor_tensor(out=lg[:], in0=pl[:, 0:72], scalar=v("rstd")[:], in1=brb[:], op0=ALU.mult, op1=ALU.add), R=[pkl, "rv_rstd", "brb"], W=["rt_lg"])
            op("dve", lambda e: e.tensor_reduce(out=v("gmax")[:], in_=lg[:, 0:8], axis=AX.X, op=ALU.max), R=["rt_lg"], W=["rv_gmax"])
            op("dve", lambda e: e.tensor_scalar(out=R_["oh"][:], in0=lg[:, 0:8], scalar1=v("gmax")[:], scalar2=None, op0=ALU.is_equal), R=["rt_lg", "rv_gmax"], W=["rt_oh"])
            op("dve", lambda e: e.tensor_scalar(out=v("ngmax")[:], in0=v("gmax")[:], scalar1=-1.0, scalar2=None, op0=ALU.mult), R=["rv_gmax"], W=["rv_ngmax"])
            op("act", lambda e: e.activation(out=R_["eg"][:], in_=lg[:, 0:8], func=AF.Exp, bias=v("ngmax")[:], accum_out=v("sume")[:]), R=["rt_lg", "rv_ngmax"], W=["rt_eg", "rv_sume"])
            op("dve", lambda e: e.reciprocal(out=v("gp")[:], in_=v("sume")[:]), R=["rv_sume"], W=["rv_gp"])
            for g in range(8):
                if g == 0:
                    op("dve", lambda e: e.tensor_scalar(out=R_["sel"][:], in0=lg[:, 8:16], scalar1=R_["oh"][:, 0:1], scalar2=None, op0=ALU.mult), R=["rt_lg", "rt_oh"], W=["rt_sel"])
                else:
                    op("dve", lambda e, g=g: e.scalar_tensor_tensor(out=R_["sel"][:], in0=lg[:, 8 + g * 8:16 + g * 8], scalar=R_["oh"][:, g:g + 1], in1=R_["sel"][:], op0=ALU.mult, op1=ALU.add), R=["rt_lg", "rt_oh", "rt_sel"], W=["rt_sel"])
            op("dve", lambda e: e.tensor_reduce(out=v("m1")[:], in_=R_["sel"][:], axis=AX.X, op=ALU.max), R=["rt_sel"], W=["rv_m1"])
            op("dve", lambda e: e.tensor_scalar(out=R_["mk1"][:], in0=R_["sel"][:], scalar1=v("m1")[:], scalar2=None, op0=ALU.is_equal), R=["rt_sel", "rv_m1"], W=["rt_mk1"])
            op("dve", lambda e: e.scalar_tensor_tensor(out=R_["sel2"][:], in0=R_["mk1"][:], scalar=-1e30, in1=R_["sel"][:], op0=ALU.mult, op1=ALU.add), R=["rt_mk1", "rt_sel"], W=["rt_sel2"])
            op("dve", lambda e: e.tensor_reduce(out=v("m2")[:], in_=R_["sel2"][:], axis=AX.X, op=ALU.max), R=["rt_sel2"], W=["rv_m2"])
            op("dve", lambda e: e.tensor_scalar(out=R_["mk2"][:], in0=R_["sel2"][:], scalar1=v("m2")[:], scalar2=None, op0=ALU.is_equal), R=["rt_sel2", "rv_m2"], W=["rt_mk2"])
            op("dve", lambda e: e.tensor_tensor(out=v("dm")[:], in0=v("m2")[:], in1=v("m1")[:], op=ALU.subtract), R=["rv_m1", "rv_m2"], W=["rv_dm"])
            op("act", lambda e: e.activation(out=v("dm")[:], in_=v("dm")[:], func=AF.Exp), R=["rv_dm"], W=["rv_dm"])
            op("dve", lambda e: e.tensor_scalar(out=v("w1")[:], in0=v("dm")[:], scalar1=1.0, scalar2=None, op0=ALU.add), R=["rv_dm"], W=["rv_w1"])
            op("dve", lambda e: e.reciprocal(out=v("w1")[:], in_=v("w1")[:]), R=["rv_w1"], W=["rv_w1"])
            op("dve", lambda e: e.tensor_tensor(out=v("w1")[:], in0=v("w1")[:], in1=v("gp")[:], op=ALU.mult), R=["rv_w1", "rv_gp"], W=["rv_w1"])
            op("dve", lambda e: e.tensor_tensor(out=v("w2")[:], in0=v("w1")[:], in1=v("dm")[:], op=ALU.mult), R=["rv_w1", "rv_dm"], W=["rv_w2"])
            op("dve", lambda e: e.tensor_scalar(out=R_["g8"][:], in0=R_["mk1"][:], scalar1=v("w1")[:], scalar2=None, op0=ALU.mult), R=["rt_mk1", "rv_w1"], W=["rt_g8"])
            op("dve", lambda e: e.scalar_tensor_tensor(out=R_["g8"][:], in0=R_["mk2"][:], scalar=v("w2")[:], in1=R_["g8"][:], op0=ALU.mult, op1=ALU.add), R=["rt_mk2", "rv_w2", "rt_g8"], W=["rt_g8"])
            pp, pkp = ps()
            op("pe", lambda e, pp=pp: e.matmul(pp[:, 0:8], ltri[:], R_["oh"][:], start=True, stop=False), R=["ltri", "rt_oh"], W=[pkp])
            op("pe", lambda e, pp=pp: e.matmul(pp[:, 0:8], onesf[:], ohacc[:], start=False, stop=True), R=["onesf2", "ohacc"], W=[pkp])
            op("dve", lambda e, pp=pp: e.tensor_tensor(out=R_["pos"][:], in0=pp[:, 0:8], in1=gbase[:], op=ALU.add), R=[pkp, "gbase"], W=["rt_pos"])
            op("dve", lambda e: e.tensor_tensor(out=R_["pos"][:], in0=R_["pos"][:], in1=R_["oh"][:], op=ALU.mult), R=["rt_pos", "rt_oh"], W=["rt_pos"])
            op("dve", lambda e: e.tensor_reduce(out=v("dest")[:], in_=R_["pos"][:], axis=AX.X, op=ALU.add), R=["rt_pos"], W=["rv_dest"])
            op("dve", lambda e, col=col: e.tensor_copy(out=dest_i[:, col:col + 1], in_=v("dest")[:]), R=["rv_dest"], W=["dest_i"])
            op("dve", lambda e: e.tensor_tensor(out=ohacc[:], in0=ohacc[:], in1=R_["oh"][:], op=ALU.add), R=["ohacc", "rt_oh", pkp], W=["ohacc"])
            s.dmafn("pool", lambda e, col=col: e.indirect_dma_start(out=hn_scr, out_offset=bass.IndirectOffsetOnAxis(ap=dest_i[:, col:col + 1], axis=0), in_=hn[:], in_offset=None), R=["hn", "dest_i"], W=["hn_scr"], stream="s")
            s.dmafn("pool", lambda e, col=col: e.indirect_dma_start(out=gt_scr, out_offset=bass.IndirectOffsetOnAxis(ap=dest_i[:, col:col + 1], axis=0), in_=R_["g8"][:], in_offset=None), R=["rt_g8", "dest_i"], W=["gt_scr"], stream="s")
        phase_end()
        st.close()

    def moe_phase():
        st = contextlib.ExitStack()
        hntok = sb(st, "hntok", [128, 3, D], BF16); hnT = sb(st, "hnT", [128, 16, CAP], BF16)
        gts = sb(st, "gts", [128, 3, 8]); yacc = sb(st, "yacc", [128, 3, D])
        act_ = sb(st, "actt", [128, 4, CAP], BF16); sl = sb(st, "silu", [128, CAP])
        WG = [sb(st, f"wg{i}", [128, 16, 512], BF16) for i in range(2)]
        WU = [sb(st, f"wu{i}", [128, 16, 512], BF16) for i in range(2)]
        WD = [sb(st, f"wd{i}", [128, 4, D], BF16) for i in range(2)]
        for g in range(8):
            for st3 in range(3):
                r0 = g * CAP + st3 * 128
                dma("sp", hntok[:, st3, :], hn_scr[r0:r0 + 128, :], R=["hn_scr"], W=["hntok"], stream="m")
                dma("sp", gts[:, st3, :], gt_scr[r0:r0 + 128, :], R=["gt_scr"], W=["gts"], stream="m")
            for dc in range(16):
                pt, pk = ps()
                for st3 in range(3):
                    op("pe", lambda e, st3=st3, dc=dc, pt=pt: e.matmul(pt[:, st3 * 128:(st3 + 1) * 128], hntok[:, st3, dc * 128:(dc + 1) * 128], ident[:], start=True, stop=True), R=["hntok", "identb"], W=[pk])
                op("act", lambda e, dc=dc, pt=pt: e.activation(out=hnT[:, dc, :], in_=pt[:, 0:CAP], func=AF.Copy), R=[pk], W=["hnT"])
            for e8 in range(8):
                E = g * 8 + e8
                b = E % 2
                dma("pool", WG[b][:], I["w_gate"][E].rearrange("(c p) n -> p c n", p=128), W=[f"wg{b}"], stream="e")
                dma("pool", WU[b][:], I["w_up"][E].rearrange("(c p) n -> p c n", p=128), W=[f"wu{b}"], stream="e")
                dma("pool", WD[b][:], I["w_down"][E].rearrange("(c p) n -> p c n", p=128), W=[f"wd{b}"], stream="e")
                for hc in range(4):
                    pg, pkg = ps()
                    pu, pku = ps()
                    for dc in range(16):
                        op("pe", lambda e, dc=dc, hc=hc, pg=pg, b=b: e.matmul(pg[:, 0:CAP], WG[b][:, dc, hc * 128:(hc + 1) * 128], hnT[:, dc, :], start=(dc == 0), stop=(dc == 15)), R=[f"wg{b}", "hnT"], W=[pkg])
                    for dc in range(16):
                        op("pe", lambda e, dc=dc, hc=hc, pu=pu, b=b: e.matmul(pu[:, 0:CAP], WU[b][:, dc, hc * 128:(hc + 1) * 128], hnT[:, dc, :], start=(dc == 0), stop=(dc == 15)), R=[f"wu{b}", "hnT"], W=[pku])
                    op("act", lambda e, pg=pg: e.activation(out=sl[:], in_=pg[:, 0:CAP], func=AF.Silu), R=[pkg], W=["silu"])
                    op("dve", lambda e, hc=hc, pu=pu: e.tensor_tensor(out=act_[:, hc, :], in0=sl[:], in1=pu[:, 0:CAP], op=ALU.mult), R=["silu", pku], W=["actt"])
                for st3 in range(3):
                    for db in range(4):
                        pd, pkd = ps()
                        for hc in range(4):
                            op("pe", lambda e, hc=hc, st3=st3, db=db, pd=pd, b=b: e.matmul(pd[:], act_[:, hc, st3 * 128:(st3 + 1) * 128], WD[b][:, hc, db * 512:(db + 1) * 512], start=(hc == 0), stop=(hc == 3)), R=["actt", f"wd{b}"], W=[pkd])
                        if e8 == 0:
                            op("dve", lambda e, st3=st3, db=db, pd=pd, e8=e8: e.tensor_scalar(out=yacc[:, st3, db * 512:(db + 1) * 512], in0=pd[:], scalar1=gts[:, st3, e8:e8 + 1], scalar2=None, op0=ALU.mult), R=[pkd, "gts"], W=["yacc"])
                        else:
                            op("dve", lambda e, st3=st3, db=db, pd=pd, e8=e8: e.scalar_tensor_tensor(out=yacc[:, st3, db * 512:(db + 1) * 512], in0=pd[:], scalar=gts[:, st3, e8:e8 + 1], in1=yacc[:, st3, db * 512:(db + 1) * 512], op0=ALU.mult, op1=ALU.add), R=[pkd, "gts", "yacc"], W=["yacc"])
            for st3 in range(3):
                r0 = g * CAP + st3 * 128
                dma("sp", y_scr[r0:r0 + 128, :], yacc[:, st3, :], R=["yacc"], W=["y_scr"], stream="m")
        phase_end()
        st.close()
        st = contextlib.ExitStack()
        ym = sb(st, "ym", [128, D]); hr = sb(st, "hr2", [128, D]); junk = sb(st, "junk2", [128, D], BF16)
        gf = sb(st, "gfin", [128, D]); ssq = sb(st, "fssq", [128, 1]); rs = sb(st, "frs", [128, 1])
        dma("sp", gf[:], I["gfin_bc"], W=["gfin"], stream="c")
        for tt in range(16):
            r0 = tt * 128
            s.dmafn("pool", lambda e, tt=tt: e.indirect_dma_start(out=ym[:], out_offset=None, in_=y_scr, in_offset=bass.IndirectOffsetOnAxis(ap=dest_i[:, tt:tt + 1], axis=0)), R=["y_scr", "dest_i"], W=["ym"], stream="s")
            dma("sp", hr[:], h_scr[r0:r0 + 128, :], R=["h_scr"], W=["hr2"], stream="h")
            op("dve", lambda e: e.tensor_tensor(out=hr[:], in0=hr[:], in1=ym[:], op=ALU.add), R=["hr2", "ym"], W=["hr2"])
            op("act", lambda e: e.activation(out=junk[:], in_=hr[:], func=AF.Square, accum_out=ssq[:]), R=["hr2"], W=["junk2", "fssq"])
            op("act", lambda e: e.activation(out=rs[:], in_=ssq[:], func=AF.Sqrt, scale=1.0 / D, bias=1e-6), R=["fssq"], W=["frs"])
            op("dve", lambda e: e.reciprocal(out=rs[:], in_=rs[:]), R=["frs"], W=["frs"])
            op("dve", lambda e: e.scalar_tensor_tensor(out=ym[:], in0=hr[:], scalar=rs[:], in1=gf[:], op0=ALU.mult, op1=ALU.mult), R=["hr2", "frs", "gfin"], W=["ym"])
            dma("sp", out[r0:r0 + 128, :], ym[:], R=["ym"], W=["out"], stream="o")
        phase_end()
        st.close()

    for p in range(NPASS):
        kA, s5_pass = mixer_pass(p)
        if "s5" in stages:
            s5_pass()
        if "rwkv" in stages:
            rwkv_pass(p, kA)
        if dbg and "mixed" in DBG and p == 2:
            if "rwkv" not in stages:
                op("dve", lambda e: e.memset(mixed[:, 8:16, :], 0.0), W=["mixed"])
            if "s5" not in stages:
                op("dve", lambda e: e.memset(mixed[:, 0:8, :], 0.0), W=["mixed"])
            dma("pool", DBG["mixed"], mixed[:].rearrange("p a b -> p (a b)"), R=["mixed"], stream="dbg")
        phase_end()
        kA["stA"].close()
        if p >= 2 and "moe" in stages:
            outproj(p - 2)
    phase_end()
    stM.close()
    if "moe" in stages:
        moe_phase()
    phase_end()
    es.close()
    return nc


def prep_inputs(inp):
    f = lambda a: np.ascontiguousarray(np.asarray(a, dtype=np.float32))
    x = f(inp["x"])
    pc = lambda v, n: f(np.asarray(v).reshape(n, 128).T)
    bc = lambda v, p: f(np.broadcast_to(np.asarray(v).reshape(1, -1), (p, np.asarray(v).size)))
    lam_re, lam_im = inp["s5_lambda_re"][0], inp["s5_lambda_im"][0]
    st = lambda a: f(np.asarray(a).reshape(32, 128).T)
    ls = np.repeat(np.asarray(inp["s5_log_step"][0]).reshape(32, 2, 1), 64, axis=2)
    b_re, b_im = np.asarray(inp["s5_b_re"][0]), np.asarray(inp["s5_b_im"][0])
    c_re, c_im = np.asarray(inp["s5_c_re"][0]), np.asarray(inp["s5_c_im"][0])

    def expand(a_gph):
        o = np.zeros((128, 32, 128), np.float32)
        for sbk in range(32):
            bl = sbk % 4
            for gg in range(2):
                g = 2 * sbk + gg
                o[gg * 64:(gg + 1) * 64, sbk, (2 * bl + gg) * 16:(2 * bl + gg + 1) * 16] = a_gph[g]
        return o.reshape(128, 32 * 128)

    tri = np.arange(64)[:, None] < np.arange(64)[None, :]
    rep8 = lambda m: f(np.tile(m.astype(np.float32)[:, None, :], (1, 8, 1)).reshape(64, 512))
    bones = np.zeros((128, 128), np.float32); bones[:64, :64] = 1; bones[64:, 64:] = 1
    hsel = np.zeros((128, 2), np.float32); hsel[:64, 0] = 1; hsel[64:, 1] = 1
    rst = np.ones((128, NT), np.float32); rst[:, ::64] = 0
    common = {
        "gmix": pc(inp["norm_mix_g"][0], 16), "w_in": f(inp["w_in"][0]),
        "mu_bc": bc(inp["rwkv_mu"][0], 128), "lam_re": st(lam_re), "lam_im": st(lam_im), "lstep": st(ls),
        "Bx_re": expand(b_re), "Bx_im": expand(b_im),
        "Cx_re": expand(np.transpose(c_re, (0, 2, 1))), "Cx_im": expand(np.transpose(c_im, (0, 2, 1))),
        "s5d": pc(inp["s5_d"][0], 8), "w_glu": f(inp["s5_w_glu"][0]), "bglu": pc(inp["s5_b_glu"][0], 8),
        "tidx": f(np.broadcast_to(np.arange(NT, dtype=np.float32)[None, :], (128, NT))),
        "w0": pc(inp["rwkv_w0"][0], 8), "a0": pc(inp["rwkv_a0"][0], 8), "w2": f(inp["rwkv_w2"][0]),
        "a2": f(inp["rwkv_a2"][0]), "g2": f(inp["rwkv_g2"][0]), "k_k": pc(inp["rwkv_k_k"][0], 8),
        "k_a": pc(inp["rwkv_k_a"][0], 8), "r_k": pc(np.asarray(inp["rwkv_r_k"][0]).reshape(-1), 8),
        "lnw_bc": bc(inp["rwkv_ln_w"][0], 64), "lnb_bc": bc(inp["rwkv_ln_b"][0], 64),
        "w_out": f(inp["w_out"][0]), "gffn_bc": bc(inp["norm_ffn_g"][0], 128), "gffn_pc": pc(inp["norm_ffn_g"][0], 16),
        "wr": f(np.concatenate([np.asarray(inp["w_route_grp"][0]), np.asarray(inp["w_route_exp"][0])], axis=1)),
        "br_bc": bc(np.concatenate([np.asarray(inp["b_route_grp"][0]), np.asarray(inp["b_route_exp"][0])]), 128),
        "w_gate": f(inp["w_gate"][0]), "w_up": f(inp["w_up"][0]), "w_down": f(inp["w_down"][0]),
        "gfin_bc": bc(inp["norm_final_g"], 128),
        "mu_pc": pc(np.asarray(inp["rwkv_mu"][0])[:3072], 24),
        "ident": f(np.eye(128)), "maskLT": rep8(tri), "maskLE": rep8(tri | np.eye(64, dtype=bool)), "maskGT": rep8(tri.T),
        "identrep": rep8(np.eye(64)), "rstmask": rst, "bones": bones, "hsel": hsel,
        "ltri": f(np.arange(128)[:, None] < np.arange(128)[None, :]),
        "gbase": f(np.broadcast_to((np.arange(8) * CAP).astype(np.float32)[None, :], (128, 8))),
    }
    maps = []
    for c in range(8):
        b, th = c // 2, c % 2
        xT = np.zeros((D, 4096), np.float32)
        if th == 0:
            xT[:, 2048:] = x[b, :2048].T
        else:
            xT[:, :] = x[b].T
        m = dict(common)
        m["xT"] = xT
        m["xtok"] = f(x[b, th * 2048:(th + 1) * 2048])
        maps.append(m)
    return maps


_NC_CACHE = {}


def kernel(**inputs):
    maps = prep_inputs(inputs)
    if "nc" not in _NC_CACHE:
        _NC_CACHE["nc"] = build_program()
    nc = _NC_CACHE["nc"]
    res = run_bass_kernel_spmd(nc, maps, core_ids=list(range(8)))
    outp = np.zeros((4, 4096, D), np.float32)
    for c in range(8):
        b, th = c // 2, c % 2
        outp[b, th * 2048:(th + 1) * 2048] = np.asarray(res.results[c]["out"], dtype=np.float32)
    return outp
```
